# Optimizing a Trainium2 kernel written in Bass

```python
import math
import jax, jax.numpy as jnp
from jax import lax
import numpy as np

D_MODEL = 2048
BATCH = 1
SEQ = 8192
DEPTH = 1

HEAD_DIM = 128
DIFF_HEADS = D_MODEL // 512
DIFF_V_DIM = 2 * HEAD_DIM
FOX_HEADS = D_MODEL // 256
Q_BLOCK = 128
ROPE_THETA = 10000.0
N_GROUPS = 4
EXPERTS_PER_GROUP = 8
N_EXPERTS = N_GROUPS * EXPERTS_PER_GROUP
TOP_K = 2
EXPERT_FF = D_MODEL // 4
MOE_BLOCK = 128
LN_EPS = 1e-5
DEEPNORM_ALPHA = (2 * DEPTH) ** 0.25
DEEPNORM_BETA = (8 * DEPTH) ** -0.25

DIFF_QK_COLS = DIFF_HEADS * 2 * HEAD_DIM
DIFF_V_COLS = DIFF_HEADS * DIFF_V_DIM
FOX_QK_COLS = FOX_HEADS * HEAD_DIM
FOX_V_COLS = FOX_HEADS * HEAD_DIM
GATE_COLS = 2 * D_MODEL
IN_SIZES = (DIFF_QK_COLS, DIFF_QK_COLS, DIFF_V_COLS, FOX_QK_COLS, FOX_QK_COLS, FOX_V_COLS, FOX_HEADS, GATE_COLS)
IN_COLS = sum(IN_SIZES)
IN_SPLITS = tuple(np.cumsum(IN_SIZES)[:-1].tolist())
VALUE_SEGMENTS = (2, 5)

kernel_name = "hybrid_diffattn_fox_hmoe_deepnorm"


def _layer_norm(x, g, b):
    xf = x.astype(jnp.float32)
    mu = jnp.mean(xf, -1, keepdims=True)
    var = jnp.mean(jnp.square(xf - mu), -1, keepdims=True)
    y = (xf - mu) * lax.rsqrt(var + LN_EPS) * g.astype(jnp.float32) + b.astype(jnp.float32)
    return y.astype(x.dtype)


def _rope(t, cos, sin):
    half = t.shape[-1] // 2
    tf = t.astype(jnp.float32)
    t1, t2 = tf[..., :half], tf[..., half:]
    return jnp.concatenate([t1 * cos - t2 * sin, t1 * sin + t2 * cos], -1).astype(t.dtype)


def _heads(t, n):
    b, s, _ = t.shape
    return t.reshape(b, s, n, -1).transpose(0, 2, 1, 3)


def _to_blocks(t):
    b, h, s, e = t.shape
    return jnp.moveaxis(t.reshape(b, h, s // Q_BLOCK, Q_BLOCK, e), 2, 0)


def _from_blocks(t):
    nb, b, h, q, e = t.shape
    return jnp.moveaxis(t, 0, 2).reshape(b, h, nb * q, e).transpose(0, 2, 1, 3)


def _causal_block_attention(dq1, dq2, dk1, dk2, dv, lam, fq, fk, fv, fcum):
    b, hf, seq = fcum.shape
    nb = seq // Q_BLOCK
    k_pos = jnp.arange(seq)
    scale = HEAD_DIM ** -0.5
    fcum_q = jnp.moveaxis(fcum.reshape(b, hf, nb, Q_BLOCK), 2, 0)

    def probs(q, k, causal, bias=None):
        s = jnp.einsum("bhqd,bhkd->bhqk", q, k, preferred_element_type=jnp.float32) * scale
        if bias is not None:
            s = s + bias
        return jax.nn.softmax(jnp.where(causal, s, -jnp.inf), axis=-1)

    def one_block(args):
        i, q1, q2, qf, cq = args
        q_pos = i * Q_BLOCK + jnp.arange(Q_BLOCK)
        causal = q_pos[:, None] >= k_pos[None, :]
        a_diff = probs(q1, dk1, causal) - lam * probs(q2, dk2, causal)
        o_diff = jnp.einsum("bhqk,bhke->bhqe", a_diff.astype(dv.dtype), dv)
        decay = cq[..., :, None] - fcum[..., None, :]
        a_fox = probs(qf, fk, causal, decay)
        o_fox = jnp.einsum("bhqk,bhke->bhqe", a_fox.astype(fv.dtype), fv)
        return o_diff, o_fox

    o_diff, o_fox = lax.map(one_block, (jnp.arange(nb), _to_blocks(dq1), _to_blocks(dq2), _to_blocks(fq), fcum_q))
    return _from_blocks(o_diff), _from_blocks(o_fox)


def _hybrid_mixer(h, w_in, b_forget, lam_q1, lam_k1, lam_q2, lam_k2, diff_norm_g,
                  w_proj_diff, w_proj_fox, w_out, lam_init):
    f32 = jnp.float32
    b, s, _ = h.shape
    dq, dk, dv, fq, fk, fv, f_logit, gate_logit = jnp.split(h @ w_in, IN_SPLITS, axis=-1)
    inv_freq = ROPE_THETA ** (-jnp.arange(0, HEAD_DIM, 2, dtype=f32) / HEAD_DIM)
    ang = jnp.arange(s, dtype=f32)[:, None] * inv_freq[None, :]
    cos, sin = jnp.cos(ang), jnp.sin(ang)
    dq = _rope(_heads(dq, 2 * DIFF_HEADS), cos, sin)
    dk = _rope(_heads(dk, 2 * DIFF_HEADS), cos, sin)
    dq1, dq2 = dq[:, 0::2], dq[:, 1::2]
    dk1, dk2 = dk[:, 0::2], dk[:, 1::2]
    dv = _heads(dv, DIFF_HEADS)
    lam = (jnp.exp(jnp.sum(lam_q1.astype(f32) * lam_k1.astype(f32)))
           - jnp.exp(jnp.sum(lam_q2.astype(f32) * lam_k2.astype(f32))) + lam_init)
    fq, fk, fv = _heads(fq, FOX_HEADS), _heads(fk, FOX_HEADS), _heads(fv, FOX_HEADS)
    log_f = jax.nn.log_sigmoid(f_logit.astype(f32) + b_forget.astype(f32))
    fcum = jnp.cumsum(log_f, axis=1).transpose(0, 2, 1)
    o_diff, o_fox = _causal_block_attention(dq1, dq2, dk1, dk2, dv, lam, fq, fk, fv, fcum)
    of = o_diff.astype(f32)
    of = of * lax.rsqrt(jnp.mean(jnp.square(of), -1, keepdims=True) + LN_EPS) * diff_norm_g.astype(f32) * (1.0 - lam_init)
    u_diff = of.astype(h.dtype).reshape(b, s, DIFF_V_COLS) @ w_proj_diff
    u_fox = o_fox.reshape(b, s, FOX_V_COLS) @ w_proj_fox
    g_diff, g_fox = jnp.split(jax.nn.sigmoid(gate_logit), 2, axis=-1)
    return (g_diff * u_diff + g_fox * u_fox) @ w_out


def _hierarchical_moe(h, w_rg, b_rg, w_re, b_re, w_gate, w_up, w_down):
    f32 = jnp.float32
    b, s, d = h.shape
    t = b * s
    xt = h.reshape(t, d)
    p_group = jax.nn.softmax((xt @ w_rg).astype(f32) + b_rg.astype(f32), axis=-1)
    top_gp, top_g = lax.top_k(p_group, 1)
    exp_logits = jnp.einsum("td,gde->tge", xt, w_re).astype(f32) + b_re.astype(f32)
    sel_logits = exp_logits[jnp.arange(t), top_g[:, 0]]
    top_el, top_e = lax.top_k(sel_logits, TOP_K)
    weights = top_gp * jax.nn.softmax(top_el, axis=-1)
    expert_ids = top_g * EXPERTS_PER_GROUP + top_e
    a = t * TOP_K
    flat_e = expert_ids.reshape(a)
    flat_tok = jnp.repeat(jnp.arange(t), TOP_K)
    order = jnp.argsort(flat_e)
    s_e, s_tok, s_w = flat_e[order], flat_tok[order], weights.reshape(a)[order]
    counts = jnp.bincount(flat_e, length=N_EXPERTS)
    group_start = jnp.cumsum(counts) - counts
    padded = (counts + MOE_BLOCK - 1) // MOE_BLOCK * MOE_BLOCK
    pad_end = jnp.cumsum(padded)
    pad_start = pad_end - padded
    dest = pad_start[s_e] + jnp.arange(a) - group_start[s_e]
    n_blocks = -(-a // MOE_BLOCK) + N_EXPERTS
    rows = n_blocks * MOE_BLOCK
    xs = jnp.zeros((rows, d), xt.dtype).at[dest].set(xt[s_tok])
    block_expert = jnp.minimum(jnp.searchsorted(pad_end, jnp.arange(n_blocks) * MOE_BLOCK, side="right"), N_EXPERTS - 1)

    def expert_block(args):
        xb, e = args
        return (jax.nn.silu(xb @ w_gate[e]) * (xb @ w_up[e])) @ w_down[e]

    ys = lax.map(expert_block, (xs.reshape(n_blocks, MOE_BLOCK, d), block_expert)).reshape(rows, d)
    contrib = ys[dest] * s_w[:, None].astype(ys.dtype)
    return jax.ops.segment_sum(contrib, s_tok, num_segments=t).reshape(b, s, d)


def setup_inputs(seed: int = 0) -> dict:
    key = jax.random.key(seed)
    ks = jax.random.split(key, 24)
    f32 = jnp.float32

    def nrm(k, shape, scale):
        return jax.random.normal(k, shape, f32) * scale

    col_scale = jnp.concatenate([jnp.full((n,), DEEPNORM_BETA if i in VALUE_SEGMENTS else 1.0, f32)
                                 for i, n in enumerate(IN_SIZES)])
    return {
        "x": nrm(ks[0], (BATCH, SEQ, D_MODEL), 1.0),
        "w_in": nrm(ks[1], (DEPTH, D_MODEL, IN_COLS), D_MODEL ** -0.5) * col_scale,
        "b_forget": jax.random.uniform(ks[2], (DEPTH, FOX_HEADS), f32, 1.0, 4.0),
        "lam_q1": nrm(ks[3], (DEPTH, HEAD_DIM), 0.1),
        "lam_k1": nrm(ks[4], (DEPTH, HEAD_DIM), 0.1),
        "lam_q2": nrm(ks[5], (DEPTH, HEAD_DIM), 0.1),
        "lam_k2": nrm(ks[6], (DEPTH, HEAD_DIM), 0.1),
        "diff_norm_g": 1.0 + nrm(ks[7], (DEPTH, DIFF_HEADS, DIFF_V_DIM), 0.02),
        "w_proj_diff": nrm(ks[8], (DEPTH, DIFF_V_COLS, D_MODEL), DIFF_V_COLS ** -0.5 * DEEPNORM_BETA),
        "w_proj_fox": nrm(ks[9], (DEPTH, FOX_V_COLS, D_MODEL), FOX_V_COLS ** -0.5 * DEEPNORM_BETA),
        "w_out": nrm(ks[10], (DEPTH, D_MODEL, D_MODEL), D_MODEL ** -0.5 * DEEPNORM_BETA),
        "ln1_g": 1.0 + nrm(ks[11], (DEPTH, D_MODEL), 0.02),
        "ln1_b": nrm(ks[12], (DEPTH, D_MODEL), 0.02),
        "w_router_group": nrm(ks[13], (DEPTH, D_MODEL, N_GROUPS), D_MODEL ** -0.5),
        "b_router_group": nrm(ks[14], (DEPTH, N_GROUPS), 0.01),
        "w_router_expert": nrm(ks[15], (DEPTH, N_GROUPS, D_MODEL, EXPERTS_PER_GROUP), D_MODEL ** -0.5),
        "b_router_expert": nrm(ks[16], (DEPTH, N_GROUPS, EXPERTS_PER_GROUP), 0.01),
        "w_gate": nrm(ks[17], (DEPTH, N_EXPERTS, D_MODEL, EXPERT_FF), D_MODEL ** -0.5),
        "w_up": nrm(ks[18], (DEPTH, N_EXPERTS, D_MODEL, EXPERT_FF), D_MODEL ** -0.5 * DEEPNORM_BETA),
        "w_down": nrm(ks[19], (DEPTH, N_EXPERTS, EXPERT_FF, D_MODEL), EXPERT_FF ** -0.5 * DEEPNORM_BETA),
        "ln2_g": 1.0 + nrm(ks[20], (DEPTH, D_MODEL), 0.02),
        "ln2_b": nrm(ks[21], (DEPTH, D_MODEL), 0.02),
    }


def reference(x, w_in, b_forget, lam_q1, lam_k1, lam_q2, lam_k2, diff_norm_g, w_proj_diff, w_proj_fox,
              w_out, ln1_g, ln1_b, w_router_group, b_router_group, w_router_expert, b_router_expert,
              w_gate, w_up, w_down, ln2_g, ln2_b):
    for layer in range(DEPTH):
        lam_init = 0.8 - 0.6 * math.exp(-0.3 * layer)
        mix = _hybrid_mixer(x, w_in[layer], b_forget[layer], lam_q1[layer], lam_k1[layer], lam_q2[layer],
                            lam_k2[layer], diff_norm_g[layer], w_proj_diff[layer], w_proj_fox[layer],
                            w_out[layer], lam_init)
        x = _layer_norm(DEEPNORM_ALPHA * x + mix, ln1_g[layer], ln1_b[layer])
        ffn = _hierarchical_moe(x, w_router_group[layer], b_router_group[layer], w_router_expert[layer],
                                b_router_expert[layer], w_gate[layer], w_up[layer], w_down[layer])
        x = _layer_norm(DEEPNORM_ALPHA * x + ffn, ln2_g[layer], ln2_b[layer])
    return x
```

```python
import math
from contextlib import ExitStack

import numpy as np
import concourse.bass as bass
import concourse.mybir as mybir
from concourse.bass_utils import run_bass_kernel_spmd

F32 = mybir.dt.float32
BF16 = mybir.dt.bfloat16
I32 = mybir.dt.int32
AF = mybir.ActivationFunctionType
ALU = mybir.AluOpType
AX = mybir.AxisListType

D = 2048
S = 8192
NB = S // 128
NQ = S // 512
NCORE = 8
HD = 128
SCALE = HD ** -0.5
NEG = -30000.0
LN_EPS = 1e-5
ALPHA = 2.0 ** 0.25
LAM_INIT = 0.2


class Buf:
    __slots__ = ("w", "r")

    def __init__(self):
        self.w = None
        self.r = {}


class Eng:
    def __init__(self, nc, eng, name, es, strict=True):
        self.eng = eng
        self.name = name
        self.sem = es.enter_context(nc.semaphore("sem_" + name))
        self.cnt = 0
        self.waited = {}
        self.strict = strict

    def need(self, tok):
        if tok is None:
            return
        sem, val, owner = tok
        if owner is self and not self.strict:
            return
        key = id(sem)
        if self.waited.get(key, 0) >= val:
            return
        self.eng.wait_ge(sem, val)
        self.waited[key] = val


class KB:
    def __init__(self, nc, es, ndma=40):
        self.nc = nc
        self.es = es
        self.pe = Eng(nc, nc.tensor, "pe", es, strict=False)
        self.act = Eng(nc, nc.scalar, "act", es)
        self.dve = Eng(nc, nc.vector, "dve", es)
        self.pool = Eng(nc, nc.gpsimd, "pool", es)
        self.sp = Eng(nc, nc.sync, "sp", es)
        self.dsem = [es.enter_context(nc.semaphore("dma%d" % i)) for i in range(ndma)]
        self.dcnt = [0] * ndma
        self.di = {"hw": 0, "sw": ndma // 2}
        self.out_toks = []

    def _pre(self, E, reads, writes):
        for b in reads:
            E.need(b.w)
        for b in writes:
            E.need(b.w)
            for t in b.r.values():
                E.need(t)

    def _post(self, tok, reads, writes):
        for b in reads:
            k = id(tok[0])
            old = b.r.get(k)
            if old is None or old[1] < tok[1]:
                b.r[k] = tok
        for b in writes:
            b.w = tok
            b.r = {}

    def op(self, E, fn, reads=(), writes=()):
        self._pre(E, reads, writes)
        inst = fn()
        E.cnt += 1
        inst.then_inc(E.sem, 1)
        tok = (E.sem, E.cnt, E)
        self._post(tok, reads, writes)
        return tok

    def dma(self, E, out, in_, reads=(), writes=(), **kw):
        self._pre(E, reads, writes)
        half = len(self.dsem) // 2
        if E is self.pool:
            i = self.di["sw"]
            self.di["sw"] = half + (i + 1 - half) % half
        else:
            i = self.di["hw"]
            self.di["hw"] = (i + 1) % half
        sem = self.dsem[i]
        if self.dcnt[i] > 0:
            E.need((sem, self.dcnt[i], None))
        inst = E.eng.dma_start(out=out, in_=in_, **kw)
        self.dcnt[i] += 16
        inst.then_inc(sem, 16)
        tok = (sem, self.dcnt[i], None)
        self._post(tok, reads, writes)
        return tok

    def barrier(self):
        engs = [self.pe, self.act, self.dve, self.pool, self.sp]
        for E in engs:
            for Fe in engs:
                if Fe is not E and Fe.cnt > 0:
                    E.need((Fe.sem, Fe.cnt, Fe))
            for i, c in enumerate(self.dcnt):
                if c > 0:
                    E.need((self.dsem[i], c, None))

    def sb(self, name, shape, dt):
        return self.es.enter_context(self.nc.sbuf_tensor("s_" + name, shape, dt))

    def ps(self, name, shape, dt=F32):
        return self.es.enter_context(self.nc.psum_tensor("p_" + name, shape, dt))


def build_l1():
    nc = bass.Bass("TRN2", target_bir_lowering=False)
    xTb = nc.dram_tensor("xTb", [NB, 128, 16, 128], F32, kind="ExternalInput").ap()
    wq_d = nc.dram_tensor("wq", [128, 16 * 768], F32, kind="ExternalInput").ap()
    wv_d = nc.dram_tensor("wv", [128, 16 * 385], F32, kind="ExternalInput").ap()
    cs_d = nc.dram_tensor("cs", [NB, 128, 256], F32, kind="ExternalInput").ap()
    negb_d = nc.dram_tensor("negb", [128, 1], F32, kind="ExternalInput").ap()
    cmask_d = nc.dram_tensor("cmask", [128, 4 * 512], F32, kind="ExternalInput").ap()
    cmat_d = nc.dram_tensor("cmat", [128, 4 * 128], F32, kind="ExternalInput").ap()
    of_d = nc.dram_tensor("o_f", [S, 128], F32, kind="ExternalOutput").ap()
    od_d = nc.dram_tensor("o_d", [S, 256], F32, kind="ExternalOutput").ap()

    with ExitStack() as es:
        kb = KB(nc, es)
        pe, act, dve, pool, sp = kb.pe, kb.act, kb.dve, kb.pool, kb.sp

        fqT = kb.sb("fqT", [128, S], BF16)
        fkT = kb.sb("fkT", [128, S], BF16)
        dqT = kb.sb("dqT", [128, S], BF16)
        dkT = kb.sb("dkT", [128, S], BF16)
        Vf = kb.sb("Vf", [128, NB, 129], BF16)
        Vd = kb.sb("Vd", [128, NB, 257], BF16)
        zcol = kb.sb("zcol", [128, NB], F32)
        cmat = kb.sb("cmat", [128, 512], F32)
        identb = kb.sb("identb", [128, 128], BF16)
        maskb = kb.sb("maskb", [128, 4 * 512], BF16)
        negb = kb.sb("negb", [128, 1], F32)
        ones3 = kb.sb("ones3", [3, 128], BF16)
        b_fq = [Buf() for _ in range(NB)]
        b_fk = [Buf() for _ in range(NB)]
        b_dq = [Buf() for _ in range(NB)]
        b_dk = [Buf() for _ in range(NB)]
        b_vf = [Buf() for _ in range(NB)]
        b_vd = [Buf() for _ in range(NB)]
        b_z = Buf()
        b_cmat, b_identb, b_maskb, b_negb, b_ones3 = Buf(), Buf(), Buf(), Buf(), Buf()

        psb = [kb.ps("ps%d" % i, [128, 512]) for i in range(8)]
        b_ps = [Buf() for _ in range(8)]

        kb.dma(sp, cmat[:], cmat_d[:, :], writes=[b_cmat])
        kb.dma(sp, negb[:], negb_d[:, :], writes=[b_negb])
        kb.dma(pool, identb[:], cmat_d[:, 0:128], writes=[b_identb])
        for j in range(4):
            kb.dma(pool, maskb[:, j * 512:(j + 1) * 512], cmask_d[:, j * 512:(j + 1) * 512], writes=[b_maskb])
        kb.op(dve, lambda: nc.vector.memset(ones3[:], 1.0), writes=[b_ones3])
        kb.op(dve, lambda: nc.vector.memset(Vf[:, :, 128:129], 1.0), writes=b_vf)
        kb.op(dve, lambda: nc.vector.memset(Vd[:, :, 256:257], 1.0), writes=b_vd)

        with ExitStack() as es1:
            kb1 = kb
            old_es = kb.es
            kb.es = es1
            wq = kb.sb("wq", [128, 16 * 768], BF16)
            wv = kb.sb("wv", [128, 16 * 385], BF16)
            xs = kb.sb("xs", [128, 16 * 128], F32)
            xb = [kb.sb("xb%d" % i, [128, 16 * 128], BF16) for i in range(2)]
            cst = [kb.sb("cst%d" % i, [128, 256], F32) for i in range(2)]
            t1 = kb.sb("t1", [128, 128], F32)
            t2 = kb.sb("t2", [128, 128], F32)
            b_wq, b_wv, b_xs = Buf(), Buf(), Buf()
            b_xb = [Buf(), Buf()]
            b_cst = [Buf(), Buf()]
            b_t1, b_t2 = Buf(), Buf()
            for k in range(16):
                kb.dma(pool, wq[:, k * 768:(k + 1) * 768], wq_d[:, k * 768:(k + 1) * 768], writes=[b_wq])
            for k in range(16):
                kb.dma(pool, wv[:, k * 385:(k + 1) * 385], wv_d[:, k * 385:(k + 1) * 385], writes=[b_wv])

            def load_block(tb):
                kb.dma(sp, xs[:], xTb[tb].rearrange("p k t -> p (k t)"), writes=[b_xs])
                kb.dma(sp, cst[tb % 2][:], cs_d[tb], writes=[b_cst[tb % 2]])
                kb.op(pool, lambda: nc.gpsimd.tensor_copy(out=xb[tb % 2][:], in_=xs[:]),
                      reads=[b_xs], writes=[b_xb[tb % 2]])

            load_block(0)
            for tb in range(NB):
                if tb + 1 < NB:
                    load_block(tb + 1)
                s = tb % 2
                X = xb[s]
                bA, bB, bC = psb[3 * s], psb[3 * s + 1], psb[3 * s + 2]
                BA, BB, BC = b_ps[3 * s], b_ps[3 * s + 1], b_ps[3 * s + 2]
                for m in range(6):
                    bank, bb = (bA, BA) if m < 4 else (bB, BB)
                    col = (m % 4) * 128
                    for k in range(16):
                        kb.op(pe, lambda k=k, m=m, bank=bank, col=col: nc.tensor.matmul(
                            bank[:, col:col + 128], lhsT=wq[:, k * 768 + m * 128:k * 768 + (m + 1) * 128],
                            rhs=X[:, k * 128:(k + 1) * 128], start=(k == 0), stop=(k == 15)),
                            reads=[b_wq, b_xb[s]], writes=[bb])
                for k in range(16):
                    kb.op(pe, lambda k=k: nc.tensor.matmul(
                        bC[:, 0:385], lhsT=X[:, k * 128:(k + 1) * 128], rhs=wv[:, k * 385:(k + 1) * 385],
                        start=(k == 0), stop=(k == 15)), reads=[b_wv, b_xb[s]], writes=[BC])
                tsl = slice(tb * 128, (tb + 1) * 128)
                C0 = cst[s][:, 0:128]
                C1 = cst[s][:, 128:256]
                kb.op(dve, lambda: nc.vector.scalar_tensor_tensor(out=t1[:], in0=bA[:, 0:128], scalar=SCALE, in1=C0,
                                                                  op0=ALU.mult, op1=ALU.mult),
                      reads=[BA, b_cst[s]], writes=[b_t1])
                kb.op(dve, lambda: nc.vector.scalar_tensor_tensor(out=t2[:], in0=bA[:, 128:256], scalar=SCALE, in1=C1,
                                                                  op0=ALU.mult, op1=ALU.mult),
                      reads=[BA, b_cst[s]], writes=[b_t2])
                kb.op(dve, lambda: nc.vector.tensor_tensor(out=dqT[:, tsl], in0=t1[:], in1=t2[:], op=ALU.add),
                      reads=[b_t1, b_t2], writes=[b_dq[tb]])
                kb.op(dve, lambda: nc.vector.tensor_tensor(out=t1[:], in0=bA[:, 256:384], in1=C0, op=ALU.mult),
                      reads=[BA, b_cst[s]], writes=[b_t1])
                kb.op(dve, lambda: nc.vector.tensor_tensor(out=t2[:], in0=bA[:, 384:512], in1=C1, op=ALU.mult),
                      reads=[BA, b_cst[s]], writes=[b_t2])
                kb.op(dve, lambda: nc.vector.tensor_tensor(out=dkT[:, tsl], in0=t1[:], in1=t2[:], op=ALU.add),
                      reads=[b_t1, b_t2], writes=[b_dk[tb]])
                kb.op(act, lambda: nc.scalar.activation(out=fqT[:, tsl], in_=bB[:, 0:128], func=AF.Copy, scale=SCALE),
                      reads=[BB], writes=[b_fq[tb]])
                kb.op(act, lambda: nc.scalar.activation(out=fkT[:, tsl], in_=bB[:, 128:256], func=AF.Copy),
                      reads=[BB], writes=[b_fk[tb]])
                kb.op(act, lambda: nc.scalar.activation(out=Vd[:, tb, 0:256], in_=bC[:, 0:256], func=AF.Copy),
                      reads=[BC], writes=[b_vd[tb]])
                kb.op(act, lambda: nc.scalar.activation(out=Vf[:, tb, 0:128], in_=bC[:, 256:384], func=AF.Copy),
                      reads=[BC], writes=[b_vf[tb]])
                kb.op(act, lambda: nc.scalar.activation(out=zcol[:, tb:tb + 1], in_=bC[:, 384:385], func=AF.Copy),
                      reads=[BC], writes=[b_z])
            kb.barrier()
            kb.es = old_es

        cmatb = kb.sb("cmatb", [128, 512], BF16)
        onesb = kb.sb("onesb", [128, 128], BF16)
        b_cmatb, b_onesb = Buf(), Buf()
        kb.dma(pool, cmatb[:], cmat_d[:, :], writes=[b_cmatb])
        kb.op(dve, lambda: nc.vector.memset(onesb[:], 1.0), writes=[b_onesb])
        identc = cmatb[:, 0:128]
        trib = cmatb[:, 128:256]
        e0b = cmatb[:, 256:384]
        gselb = cmatb[:, 384:512]
        spc = kb.sb("spc", [128, NB], F32)
        ones64 = kb.sb("ones64", [128, NB], F32)
        tot = kb.sb("tot", [128, NB], F32)
        cinc = kb.sb("cinc", [128, NB], F32)
        ccol = kb.sb("ccol", [128, NB], F32)
        crefb = kb.sb("crefb", [128, NQ], F32)
        biasK = kb.sb("biasK", [128, NQ * NB], F32)
        cT = kb.sb("cT", [NB, 128], F32)
        crefT = kb.sb("crefT", [NB, 1], F32)
        crel = kb.sb("crel", [NB, 128], F32)
        crel3 = kb.sb("crel3", [3, S], BF16)
        b_spc, b_ones64, b_tot, b_cinc, b_ccol, b_crefb, b_biasK = (Buf() for _ in range(7))
        b_cT, b_crefT, b_crel, b_crel3 = (Buf() for _ in range(4))
        st1 = kb.sb("st1", [128, 128], F32)
        st2 = kb.sb("st2", [128, 128], F32)
        b_st1, b_st2 = Buf(), Buf()

        def split3(name, src, b_src, P_, N_):
            parts = [kb.sb("%s_p%d" % (name, i), [P_, N_], BF16) for i in range(3)]
            bp = [Buf() for _ in range(3)]
            V = nc.vector
            kb.op(dve, lambda: V.tensor_copy(out=parts[0][:], in_=src), reads=[b_src], writes=[bp[0]])
            kb.op(dve, lambda: V.tensor_tensor(out=st1[0:P_, 0:N_], in0=src, in1=parts[0][:], op=ALU.subtract),
                  reads=[b_src, bp[0]], writes=[b_st1])
            kb.op(dve, lambda: V.tensor_copy(out=parts[1][:], in_=st1[0:P_, 0:N_]), reads=[b_st1], writes=[bp[1]])
            kb.op(dve, lambda: V.tensor_tensor(out=st2[0:P_, 0:N_], in0=st1[0:P_, 0:N_], in1=parts[1][:], op=ALU.subtract),
                  reads=[b_st1, bp[1]], writes=[b_st2])
            kb.op(dve, lambda: V.tensor_copy(out=parts[2][:], in_=st2[0:P_, 0:N_]), reads=[b_st2], writes=[bp[2]])
            return parts, bp

        def mm3(out_ap, bout, lhs_fn, rhs_fn, extra_reads):
            for i in range(3):
                kb.op(pe, lambda i=i: nc.tensor.matmul(out_ap, lhsT=lhs_fn(i), rhs=rhs_fn(i), start=(i == 0), stop=(i == 2)),
                      reads=extra_reads(i), writes=[bout])

        kb.op(dve, lambda: nc.vector.memset(ones64[:], 1.0), writes=[b_ones64])
        kb.op(dve, lambda: nc.vector.tensor_scalar(out=zcol[:], in0=zcol[:], scalar1=negb[:, 0:1], scalar2=None,
                                                   op0=ALU.add), reads=[b_z, b_negb], writes=[b_z])
        kb.op(act, lambda: nc.scalar.activation(out=spc[:], in_=zcol[:], func=AF.Exp, scale=-1.0),
              reads=[b_z], writes=[b_spc])
        kb.op(act, lambda: nc.scalar.activation(out=spc[:], in_=spc[:], func=AF.Ln, bias=1.0, scale=1.0),
              reads=[b_spc], writes=[b_spc])
        P0, P1 = psb[0], psb[1]
        B0, B1 = b_ps[0], b_ps[1]
        spp, bspp = split3("spp", spc[:], b_spc, 128, NB)
        mm3(P0[:, 0:NB], B0, lambda i: trib, lambda i: spp[i][:], lambda i: [b_cmatb, bspp[i]])
        mm3(P1[:, 0:NB], B1, lambda i: onesb[:], lambda i: spp[i][:], lambda i: [b_onesb, bspp[i]])
        kb.op(dve, lambda: nc.vector.tensor_copy(out=tot[:], in_=P1[:, 0:NB]), reads=[B1], writes=[b_tot])
        kb.op(dve, lambda: nc.vector.tensor_tensor_scan(out=cinc[:], data0=ones64[:], data1=tot[:], initial=0.0,
                                                        op0=ALU.mult, op1=ALU.add),
              reads=[b_ones64, b_tot], writes=[b_cinc])
        kb.op(dve, lambda: nc.vector.tensor_tensor(out=cinc[:], in0=cinc[:], in1=tot[:], op=ALU.subtract),
              reads=[b_cinc, b_tot], writes=[b_cinc])
        kb.op(dve, lambda: nc.vector.tensor_tensor(out=cinc[:], in0=cinc[:], in1=P0[:, 0:NB], op=ALU.add),
              reads=[b_cinc, B0], writes=[b_cinc])
        kb.op(dve, lambda: nc.vector.tensor_scalar(out=ccol[:], in0=cinc[:], scalar1=-1.0, scalar2=None, op0=ALU.mult),
              reads=[b_cinc], writes=[b_ccol])
        ccp, bccp = split3("ccp", ccol[:], b_ccol, 128, NB)
        mm3(P0[:, 64:64 + NQ], B0, lambda i: e0b, lambda i: ccp[i][:, 0:NB:4], lambda i: [b_cmatb, bccp[i]])
        kb.op(dve, lambda: nc.vector.tensor_copy(out=crefb[:], in_=P0[:, 64:64 + NQ]), reads=[B0], writes=[b_crefb])
        for qc in range(NQ):
            kb.op(dve, lambda qc=qc: nc.vector.tensor_scalar(
                out=biasK[:, qc * NB:(qc + 1) * NB], in0=ccol[:], scalar1=-1.0, scalar2=crefb[:, qc:qc + 1],
                op0=ALU.mult, op1=ALU.add), reads=[b_ccol, b_crefb], writes=[b_biasK])
        mm3(P1[0:NB, 128:256], B1, lambda i: ccp[i][:, 0:NB], lambda i: identc, lambda i: [b_cmatb, bccp[i]])
        kb.op(dve, lambda: nc.vector.tensor_copy(out=cT[:], in_=P1[0:NB, 128:256]), reads=[B1], writes=[b_cT])
        cTp, bcTp = split3("cTp", cT[:], b_cT, NB, 128)
        mm3(P1[0:NB, 256:257], B1, lambda i: gselb[0:NB, 0:NB], lambda i: cTp[i][:, 0:1], lambda i: [b_cmatb, bcTp[i]])
        kb.op(dve, lambda: nc.vector.tensor_copy(out=crefT[:], in_=P1[0:NB, 256:257]), reads=[B1], writes=[b_crefT])
        kb.op(dve, lambda: nc.vector.tensor_scalar(out=crel[:], in0=cT[:], scalar1=crefT[:, 0:1], scalar2=None,
                                                   op0=ALU.subtract), reads=[b_cT, b_crefT], writes=[b_crel])
        crp, bcrp = split3("crp", crel[:], b_crel, NB, 128)
        scr = nc.dram_tensor("scr_c", [3, S], BF16).ap()
        b_scr = Buf()
        for j in range(3):
            kb.dma(sp, scr[j].rearrange("(b p) -> b p", p=128), crp[j][:], reads=[bcrp[j]], writes=[b_scr])
        kb.dma(sp, crel3[:, :], scr[:, :], reads=[b_scr], writes=[b_crel3])

        Pt = [kb.sb("Pt%d" % i, [128, 512], BF16) for i in range(3)]
        b_Pt = [Buf() for _ in range(3)]
        osb = [kb.sb("osb%d" % i, [128, 256], F32) for i in range(2)]
        b_osb = [Buf(), Buf()]
        rcp = [kb.sb("rcp%d" % i, [128, 1], F32) for i in range(2)]
        b_rcp = [Buf(), Buf()]
        ocnt = [0]

        def attn_pass(qT, bq, kT, bk, V, bv, dv, use_bias, out_d):
            tiles = [(qc, kblk) for qc in range(NQ) for kblk in range(4 * qc + 4)]
            nt = len(tiles)

            def emit_S(i):
                qc, kblk = tiles[i]
                bank, bb = psb[i % 2], b_ps[i % 2]
                diag = kblk >= 4 * qc
                qsl = slice(qc * 512, (qc + 1) * 512)
                kb.op(pe, lambda: nc.tensor.matmul(bank[:, :], lhsT=kT[:, kblk * 128:(kblk + 1) * 128], rhs=qT[:, qsl],
                                                   start=True, stop=not (use_bias or diag)),
                      reads=[bk[kblk]] + bq[4 * qc:4 * qc + 4], writes=[bb])
                if use_bias:
                    kb.op(pe, lambda: nc.tensor.matmul(bank[:, :], lhsT=ones3[:, :], rhs=crel3[:, qsl],
                                                       start=False, stop=not diag),
                          reads=[b_ones3, b_crel3], writes=[bb])
                if diag:
                    j = kblk - 4 * qc
                    kb.op(pe, lambda: nc.tensor.matmul(bank[:, :], lhsT=identb[:, :], rhs=maskb[:, j * 512:(j + 1) * 512],
                                                       start=False, stop=True),
                          reads=[b_identb, b_maskb], writes=[bb])

            def emit_exp(i):
                qc, kblk = tiles[i]
                bank, bb = psb[i % 2], b_ps[i % 2]
                if use_bias:
                    kb.op(act, lambda: nc.scalar.activation(out=Pt[i % 3][:], in_=bank[:, :], func=AF.Exp,
                                                            bias=biasK[:, qc * NB + kblk:qc * NB + kblk + 1], scale=1.0),
                          reads=[bb, b_biasK], writes=[b_Pt[i % 3]])
                else:
                    kb.op(act, lambda: nc.scalar.activation(out=Pt[i % 3][:], in_=bank[:, :], func=AF.Exp),
                          reads=[bb], writes=[b_Pt[i % 3]])

            def emit_PV(i):
                qc, kblk = tiles[i]
                for sub in range(4):
                    qb = 4 * qc + sub
                    if kblk > qb:
                        continue
                    ob, bob = psb[2 + sub], b_ps[2 + sub]
                    kb.op(pe, lambda: nc.tensor.matmul(ob[:, 0:dv + 1], lhsT=Pt[i % 3][:, sub * 128:(sub + 1) * 128],
                                                       rhs=V[:, kblk, 0:dv + 1], start=(kblk == 0), stop=(kblk == qb)),
                          reads=[b_Pt[i % 3], bv[kblk]], writes=[bob])
                    if kblk == qb:
                        o = ocnt[0] % 2
                        ocnt[0] += 1
                        kb.op(dve, lambda: nc.vector.reciprocal(out=rcp[o][:], in_=ob[:, dv:dv + 1]),
                              reads=[bob], writes=[b_rcp[o]])
                        kb.op(dve, lambda: nc.vector.tensor_scalar(out=osb[o][:, 0:dv], in0=ob[:, 0:dv],
                                                                   scalar1=rcp[o][:, 0:1], scalar2=None, op0=ALU.mult),
                              reads=[bob, b_rcp[o]], writes=[b_osb[o]])
                        tok = kb.dma(pool, out_d[qb * 128:(qb + 1) * 128, :], osb[o][:, 0:dv], reads=[b_osb[o]])
                        kb.out_toks.append(tok)

            emit_S(0)
            for i in range(nt):
                if i + 1 < nt:
                    emit_S(i + 1)
                emit_exp(i)
                emit_PV(i)

        attn_pass(fqT, b_fq, fkT, b_fk, Vf, b_vf, 128, True, of_d)
        attn_pass(dqT, b_dq, dkT, b_dk, Vd, b_vd, 256, False, od_d)
        for tok in kb.out_toks:
            pool.need(tok)
    return nc


def _rope_tables():
    inv_freq = (10000.0 ** (-np.arange(0, HD, 2, dtype=np.float32) / np.float32(HD))).astype(np.float32)
    ang = np.arange(S, dtype=np.float32)[:, None] * inv_freq[None, :]
    cos = np.cos(ang).astype(np.float32).T
    sin = np.sin(ang).astype(np.float32).T
    c0 = np.concatenate([cos, cos], 0)
    c1 = np.concatenate([-sin, sin], 0)
    cs = np.stack([c0, c1], 1)
    cs = cs.reshape(128, 2, NB, 128).transpose(2, 0, 1, 3).reshape(NB, 128, 256)
    return np.ascontiguousarray(cs)


def _l1_consts():
    k = np.arange(128)[:, None]
    q = np.arange(512)[None, :]
    cmask = np.concatenate([np.where(q - k - 128 * j >= 0, 0.0, NEG) for j in range(4)], 1).astype(np.float32)
    ident = np.eye(128, dtype=np.float32)
    tri = (np.arange(128)[:, None] <= np.arange(128)[None, :]).astype(np.float32)
    e0 = np.zeros((128, 128), np.float32)
    e0[0, :] = 1.0
    g = np.zeros((128, 128), np.float32)
    for b in range(64):
        g[4 * (b // 4), b] = 1.0
    cmat = np.concatenate([ident, tri, e0, g], 1)
    return cmask, np.ascontiguousarray(cmat)


def _pk(w):
    n = w.shape[1]
    return np.ascontiguousarray(w.reshape(16, 128, n).transpose(1, 0, 2).reshape(128, 16 * n))


def run_l1(x, w_in, b_forget):
    x2 = np.asarray(x, np.float32).reshape(S, D)
    w = np.asarray(w_in, np.float32).reshape(D, -1)
    xTb = np.ascontiguousarray(x2.reshape(NB, 128, 16, 128).transpose(0, 3, 2, 1))
    cs = _rope_tables()
    cmask, cmat = _l1_consts()
    sw = np.concatenate([np.arange(64, 128), np.arange(0, 64)])
    in_maps = []
    for c in range(NCORE):
        h = c // 2
        dq = w[:, c * 128:(c + 1) * 128]
        dk = w[:, 1024 + c * 128:1024 + (c + 1) * 128]
        dv = w[:, 2048 + h * 256:2048 + (h + 1) * 256]
        fq = w[:, 3072 + c * 128:3072 + (c + 1) * 128]
        fk = w[:, 4096 + c * 128:4096 + (c + 1) * 128]
        fv = w[:, 5120 + c * 128:5120 + (c + 1) * 128]
        fl = w[:, 6144 + c:6144 + c + 1]
        wq = np.concatenate([dq, dq[:, sw], dk, dk[:, sw], fq, fk], 1)
        wv = np.concatenate([dv, fv, fl], 1)
        bfc = np.asarray(b_forget, np.float32).reshape(-1)[c]
        in_maps.append({"xTb": xTb, "wq": _pk(wq), "wv": _pk(wv), "cs": cs,
                        "negb": np.full((128, 1), 1.0, np.float32) * bfc,
                        "cmask": cmask, "cmat": cmat})
    nc = build_l1()
    res = run_bass_kernel_spmd(nc, in_maps, core_ids=list(range(NCORE)))
    o_f = [r["o_f"] for r in res.results]
    o_d = [r["o_d"] for r in res.results]
    return o_f, o_d


SKIP = set()


def build_l2(TT):
    NT = TT // 128
    NH = TT // 512
    nc = bass.Bass("TRN2", target_bir_lowering=False)
    xT_d = nc.dram_tensor("xT", [128, 16 * TT], F32, kind="ExternalInput").ap()
    x_d = nc.dram_tensor("x", [TT, D], F32, kind="ExternalInput").ap()
    o1T_d = nc.dram_tensor("o1T", [128, 8 * TT], F32, kind="ExternalInput").ap()
    o2T_d = nc.dram_tensor("o2T", [128, 8 * TT], F32, kind="ExternalInput").ap()
    ofT_d = nc.dram_tensor("ofT", [128, 8 * TT], F32, kind="ExternalInput").ap()
    lamv_d = nc.dram_tensor("lamv", [128, 512], F32, kind="ExternalInput").ap()
    gcol_d = nc.dram_tensor("gcol", [128, 8], F32, kind="ExternalInput").ap()
    wmix_d = nc.dram_tensor("wmix", [16, 128, 6144], F32, kind="ExternalInput").ap()
    wout_d = nc.dram_tensor("wout", [128, 16 * D], F32, kind="ExternalInput").ap()
    ln_d = nc.dram_tensor("lnp", [128, 4 * D], F32, kind="ExternalInput").ap()
    wr_d = nc.dram_tensor("wr", [128, 16 * 36], F32, kind="ExternalInput").ap()
    br_d = nc.dram_tensor("br", [128, 36], F32, kind="ExternalInput").ap()
    wg_d = nc.dram_tensor("wg", [32, 128, 8192], F32, kind="ExternalInput").ap()
    wu_d = nc.dram_tensor("wu", [32, 128, 8192], F32, kind="ExternalInput").ap()
    wd_d = nc.dram_tensor("wd", [32, 128, 8192], F32, kind="ExternalInput").ap()
    ident_d = nc.dram_tensor("ident", [128, 128], F32, kind="ExternalInput").ap()
    out_d = nc.dram_tensor("out", [TT, D], F32, kind="ExternalOutput").ap()
    x1_scr = nc.dram_tensor("x1_scr", [TT, D], F32).ap()
    b_scr = [Buf() for _ in range(NT)]

    with ExitStack() as es:
        kb = KB(nc, es)
        pe, act, dve, pool, sp = kb.pe, kb.act, kb.dve, kb.pool, kb.sp
        psb = [kb.ps("ps%d" % i, [128, 512]) for i in range(7)]
        b_ps = [Buf() for _ in range(7)]
        pT = kb.ps("pT", [128, 8, 128], BF16)

        ident = kb.sb("ident", [128, 128], F32)
        ones128 = kb.sb("ones128", [128, 128], BF16)
        x1Tb = kb.sb("x1Tb", [128, 16, TT], BF16)
        identb = kb.sb("identb", [128, 128], BF16)
        b_identb, b_pT = Buf(), Buf()
        coef = kb.sb("coef", [128, NT * 32], F32)
        b_ident, b_ones, b_coef, b_lnp2 = Buf(), Buf(), Buf(), Buf()
        b_x1T = [Buf() for _ in range(NT)]
        kb.dma(sp, ident[:], ident_d[:, :], writes=[b_ident])
        kb.op(dve, lambda: nc.vector.memset(ones128[:], 1.0), writes=[b_ones])
        kb.op(dve, lambda: nc.vector.tensor_copy(out=identb[:], in_=ident[:]), reads=[b_ident], writes=[b_identb])

        with ExitStack() as esB:
            kb.es = esB
            mT = kb.sb("mT", [128, 16 * TT], BF16)
            b_mT = [Buf() for _ in range(16)]
            with ExitStack() as esA:
                kb.es = esA
                xTb = kb.sb("xTb", [128, 16 * TT], BF16)
                ofTb = kb.sb("ofTb", [128, 8 * TT], BF16)
                dnT = kb.sb("dnT", [128, 8 * TT], BF16)
                lamv = kb.sb("lamv", [128, 512], F32)
                gs = kb.sb("gs", [128, 8], F32)
                prod = kb.sb("prod", [128, 128], F32)
                sv = kb.sb("sv", [128, 4], F32)
                neglam = kb.sb("neglam", [128, 1], F32)
                o1s = kb.sb("o1s", [128, TT], F32)
                o2s = kb.sb("o2s", [128, TT], F32)
                dd = [kb.sb("dd%d" % i, [128, TT], F32) for i in range(2)]
                sq = [kb.sb("sq%d" % i, [128, TT], BF16) for i in range(2)]
                vv = kb.sb("vv", [128, 512], F32)
                slab = [kb.sb("slab%d" % i, [128, 6144], BF16) for i in range(2)]
                sgd = kb.sb("sgd", [128, 512], F32)
                sgf = kb.sb("sgf", [128, 512], F32)
                m1 = kb.sb("m1", [128, 512], F32)
                m2 = kb.sb("m2", [128, 512], F32)
                b_xTb, b_ofTb, b_dnT, b_lamv, b_gs, b_prod, b_sv, b_neglam = (Buf() for _ in range(8))
                b_o1s, b_o2s, b_vv, b_sgd, b_sgf, b_m1, b_m2 = (Buf() for _ in range(7))
                b_dd = [Buf(), Buf()]
                b_sq = [Buf(), Buf()]
                b_slab = [Buf(), Buf()]

                for k in range(16):
                    kb.dma(pool, xTb[:, k * TT:(k + 1) * TT], xT_d[:, k * TT:(k + 1) * TT], writes=[b_xTb])
                for k in range(8):
                    kb.dma(pool, ofTb[:, k * TT:(k + 1) * TT], ofT_d[:, k * TT:(k + 1) * TT], writes=[b_ofTb])
                kb.dma(sp, lamv[:], lamv_d[:, :], writes=[b_lamv])
                kb.dma(sp, gs[:], gcol_d[:, :], writes=[b_gs])

                def load_slab(j):
                    for q in range(3):
                        kb.dma(pool, slab[j % 2][:, q * 2048:(q + 1) * 2048], wmix_d[j][:, q * 2048:(q + 1) * 2048],
                               writes=[b_slab[j % 2]])
                load_slab(0)

                for j in range(2 if 'A1' not in SKIP else 0):
                    kb.op(dve, lambda j=j: nc.vector.tensor_tensor(out=prod[:], in0=lamv[:, (2 * j) * 128:(2 * j + 1) * 128],
                                                                  in1=lamv[:, (2 * j + 1) * 128:(2 * j + 2) * 128], op=ALU.mult),
                          reads=[b_lamv], writes=[b_prod])
                    kb.op(dve, lambda j=j: nc.vector.reduce_sum(out=sv[:, j:j + 1], in_=prod[:], axis=AX.X),
                          reads=[b_prod], writes=[b_sv])
                kb.op(act, lambda: nc.scalar.activation(out=sv[:, 2:4], in_=sv[:, 0:2], func=AF.Exp), reads=[b_sv], writes=[b_sv])
                kb.op(dve, lambda: nc.vector.tensor_tensor(out=neglam[:], in0=sv[:, 3:4], in1=sv[:, 2:3], op=ALU.subtract),
                      reads=[b_sv], writes=[b_neglam])
                kb.op(dve, lambda: nc.vector.tensor_scalar(out=neglam[:], in0=neglam[:], scalar1=-LAM_INIT, scalar2=None,
                                                           op0=ALU.add), reads=[b_neglam], writes=[b_neglam])
                kb.op(dve, lambda: nc.vector.tensor_scalar(out=gs[:], in0=gs[:], scalar1=1.0 - LAM_INIT, scalar2=None,
                                                           op0=ALU.mult), reads=[b_gs], writes=[b_gs])
                for h in range(4 if 'A2' not in SKIP else 0):
                    for i in range(2):
                        ch = h * 2 + i
                        kb.dma(sp, o1s[:], o1T_d[:, ch * TT:(ch + 1) * TT], writes=[b_o1s])
                        kb.dma(sp, o2s[:], o2T_d[:, ch * TT:(ch + 1) * TT], writes=[b_o2s])
                        kb.op(dve, lambda i=i: nc.vector.scalar_tensor_tensor(out=dd[i][:], in0=o2s[:], scalar=neglam[:, 0:1],
                                                                               in1=o1s[:], op0=ALU.mult, op1=ALU.add),
                              reads=[b_o1s, b_o2s, b_neglam], writes=[b_dd[i]])
                        kb.op(dve, lambda i=i: nc.vector.tensor_tensor(out=sq[i][:], in0=dd[i][:], in1=dd[i][:], op=ALU.mult),
                              reads=[b_dd[i]], writes=[b_sq[i]])
                    for hf in range(NH):
                        cs_ = slice(hf * 512, (hf + 1) * 512)
                        pb, bpb = psb[hf % 2], b_ps[hf % 2]
                        for i in range(2):
                            kb.op(pe, lambda i=i: nc.tensor.matmul(pb[:, :], lhsT=ones128[:], rhs=sq[i][:, cs_],
                                                                   start=(i == 0), stop=(i == 1)),
                                  reads=[b_ones, b_sq[i]], writes=[bpb])
                        kb.op(dve, lambda: nc.vector.tensor_scalar(out=vv[:], in0=pb[:, :], scalar1=1.0 / 256.0, scalar2=LN_EPS,
                                                                   op0=ALU.mult, op1=ALU.add), reads=[bpb], writes=[b_vv])
                        kb.op(act, lambda: nc.scalar.activation(out=vv[:], in_=vv[:], func=AF.Sqrt), reads=[b_vv], writes=[b_vv])
                        kb.op(dve, lambda: nc.vector.reciprocal(out=vv[:], in_=vv[:]), reads=[b_vv], writes=[b_vv])
                        for i in range(2):
                            ch = h * 2 + i
                            kb.op(dve, lambda i=i, ch=ch: nc.vector.scalar_tensor_tensor(
                                out=dnT[:, ch * TT + hf * 512:ch * TT + (hf + 1) * 512], in0=dd[i][:, cs_],
                                scalar=gs[:, ch:ch + 1], in1=vv[:], op0=ALU.mult, op1=ALU.mult),
                                reads=[b_dd[i], b_gs, b_vv], writes=[b_dnT])
                it = 0
                for j in range(16 if 'A3' not in SKIP else 0):
                    if j + 1 < 16:
                        load_slab(j + 1)
                    sl = slab[j % 2]
                    bsl = b_slab[j % 2]
                    for hf in range(NH):
                        st = 2 * (it % 2)
                        it += 1
                        pgd, pud, pgf, puf = psb[st], psb[st + 1], psb[4], psb[5]
                        bgd, bud, bgf, buf_ = b_ps[st], b_ps[st + 1], b_ps[4], b_ps[5]
                        for k in range(16):
                            kb.op(pe, lambda k=k: nc.tensor.matmul(pgd[:, :], lhsT=sl[:, k * 128:(k + 1) * 128],
                                                                   rhs=xTb[:, k * TT + hf * 512:k * TT + (hf + 1) * 512],
                                                                   start=(k == 0), stop=(k == 15)),
                                  reads=[bsl, b_xTb], writes=[bgd])
                        for k in range(16):
                            kb.op(pe, lambda k=k: nc.tensor.matmul(pgf[:, :], lhsT=sl[:, (16 + k) * 128:(17 + k) * 128],
                                                                   rhs=xTb[:, k * TT + hf * 512:k * TT + (hf + 1) * 512],
                                                                   start=(k == 0), stop=(k == 15)),
                                  reads=[bsl, b_xTb], writes=[bgf])
                        for k in range(8):
                            kb.op(pe, lambda k=k: nc.tensor.matmul(pud[:, :], lhsT=sl[:, (32 + k) * 128:(33 + k) * 128],
                                                                   rhs=dnT[:, k * TT + hf * 512:k * TT + (hf + 1) * 512],
                                                                   start=(k == 0), stop=(k == 7)),
                                  reads=[bsl, b_dnT], writes=[bud])
                        for k in range(8):
                            kb.op(pe, lambda k=k: nc.tensor.matmul(puf[:, :], lhsT=sl[:, (40 + k) * 128:(41 + k) * 128],
                                                                   rhs=ofTb[:, k * TT + hf * 512:k * TT + (hf + 1) * 512],
                                                                   start=(k == 0), stop=(k == 7)),
                                  reads=[bsl, b_ofTb], writes=[buf_])
                        kb.op(act, lambda: nc.scalar.activation(out=sgd[:], in_=pgd[:, :], func=AF.Sigmoid),
                              reads=[bgd], writes=[b_sgd])
                        kb.op(act, lambda: nc.scalar.activation(out=sgf[:], in_=pgf[:, :], func=AF.Sigmoid),
                              reads=[bgf], writes=[b_sgf])
                        kb.op(dve, lambda: nc.vector.tensor_tensor(out=m1[:], in0=sgd[:], in1=pud[:, :], op=ALU.mult),
                              reads=[b_sgd, bud], writes=[b_m1])
                        kb.op(dve, lambda: nc.vector.tensor_tensor(out=m2[:], in0=sgf[:], in1=puf[:, :], op=ALU.mult),
                              reads=[b_sgf, buf_], writes=[b_m2])
                        kb.op(dve, lambda: nc.vector.tensor_tensor(out=mT[:, j * TT + hf * 512:j * TT + (hf + 1) * 512],
                                                                   in0=m1[:], in1=m2[:], op=ALU.add),
                              reads=[b_m1, b_m2], writes=[b_mT[j]])
                kb.barrier()
                kb.es = esB
            woutb = kb.sb("woutb", [128, 16 * D], BF16)
            lnp1 = kb.sb("lnp1", [128, 2 * D], F32)
            wr = kb.sb("wr", [128, 16 * 36], F32)
            br = kb.sb("br", [128, 36], F32)
            xt = [kb.sb("xt%d" % i, [128, D], F32) for i in range(2)]
            yv = kb.sb("yv", [128, D], F32)
            x1t = kb.sb("x1t", [128, D], F32)
            xhi = kb.sb("xhi", [128, D], BF16)
            xlo = kb.sb("xlo", [128, D], BF16)
            x1Tl = kb.sb("x1Tl", [128, 16, 128], BF16)
            wrh = kb.sb("wrh", [128, 16 * 36], BF16)
            wrl = kb.sb("wrl", [128, 16 * 36], BF16)
            wrt = kb.sb("wrt", [128, 16 * 36], F32)
            b_xhi, b_xlo, b_x1Tl, b_wrh, b_wrl, b_wrt = (Buf() for _ in range(6))
            stats = kb.sb("stats", [128, 4 * 6], F32)
            mv = kb.sb("mv", [128, 2], F32)
            rt = kb.sb("rt", [128, 64], F32)
            lg = kb.sb("lg", [128, 36], F32)
            lem = kb.sb("lem", [128, 32], F32)
            lem2 = kb.sb("lem2", [128, 32], F32)
            oh1 = kb.sb("oh1", [128, 32], F32)
            oh2 = kb.sb("oh2", [128, 32], F32)
            b_wout, b_lnp1, b_wr, b_br, b_yv, b_x1t, b_x1Tf_unused, b_stats, b_mv, b_rt, b_lg, b_lem, b_lem2, b_oh1, b_oh2 = (
                Buf() for _ in range(15))
            b_xt = [Buf(), Buf()]
            for k in range(16):
                kb.dma(pool, woutb[:, k * D:(k + 1) * D], wout_d[:, k * D:(k + 1) * D], writes=[b_wout])
            kb.dma(sp, lnp1[:], ln_d[:, 0:2 * D], writes=[b_lnp1])
            kb.dma(sp, wr[:], wr_d[:, :], writes=[b_wr])
            kb.dma(sp, br[:], br_d[:, :], writes=[b_br])
            kb.op(dve, lambda: nc.vector.tensor_copy(out=wrh[:], in_=wr[:]), reads=[b_wr], writes=[b_wrh])
            kb.op(dve, lambda: nc.vector.tensor_tensor(out=wrt[:], in0=wr[:], in1=wrh[:], op=ALU.subtract),
                  reads=[b_wr, b_wrh], writes=[b_wrt])
            kb.op(dve, lambda: nc.vector.tensor_copy(out=wrl[:], in_=wrt[:]), reads=[b_wrt], writes=[b_wrl])
            kb.dma(sp, xt[0][:], x_d[0:128, :], writes=[b_xt[0]])
            for tb in range(NT if 'B' not in SKIP else 0):
                if tb + 1 < NT:
                    kb.dma(sp, xt[(tb + 1) % 2][:], x_d[(tb + 1) * 128:(tb + 2) * 128, :], writes=[b_xt[(tb + 1) % 2]])
                X = xt[tb % 2]
                bX = b_xt[tb % 2]
                for n in range(4):
                    pb, bpb = psb[n % 2], b_ps[n % 2]
                    for k in range(16):
                        kb.op(pe, lambda k=k: nc.tensor.matmul(pb[:, :], lhsT=mT[:, k * TT + tb * 128:k * TT + (tb + 1) * 128],
                                                               rhs=woutb[:, k * D + n * 512:k * D + (n + 1) * 512],
                                                               start=(k == 0), stop=(k == 15)),
                              reads=[b_mT[k], b_wout], writes=[bpb])
                    kb.op(dve, lambda n=n: nc.vector.scalar_tensor_tensor(out=yv[:, n * 512:(n + 1) * 512],
                                                                         in0=X[:, n * 512:(n + 1) * 512], scalar=ALPHA,
                                                                         in1=pb[:, :], op0=ALU.mult, op1=ALU.add),
                          reads=[bX, bpb], writes=[b_yv])
                    kb.op(dve, lambda n=n: nc.vector.bn_stats(out=stats[:, n * 6:(n + 1) * 6], in_=yv[:, n * 512:(n + 1) * 512]),
                          reads=[b_yv], writes=[b_stats])
                _ln_tail(nc, kb, yv, b_yv, stats, b_stats, mv, b_mv, x1t, b_x1t, lnp1, b_lnp1)
                kb.dma(sp, x1_scr[tb * 128:(tb + 1) * 128, :], x1t[:], reads=[b_x1t], writes=[b_scr[tb]])
                kb.op(dve, lambda: nc.vector.tensor_copy(out=xhi[:], in_=x1t[:]), reads=[b_x1t], writes=[b_xhi])
                kb.op(dve, lambda: nc.vector.tensor_tensor(out=xlo[:], in0=x1t[:], in1=xhi[:], op=ALU.subtract),
                      reads=[b_x1t, b_xhi], writes=[b_xlo])
                for si, (src, bsrc) in enumerate(((xhi, b_xhi), (xlo, b_xlo))):
                    for k8 in range(2):
                        for kk in range(8):
                            k = k8 * 8 + kk
                            kb.op(pe, lambda k=k, kk=kk: nc.tensor.transpose(pT[:, kk, :], src[:, k * 128:(k + 1) * 128], identb[:]),
                                  reads=[bsrc, b_identb], writes=[b_pT])
                        if si == 0:
                            kb.op(act, lambda: nc.scalar.activation(out=x1Tb[:, k8 * 8:(k8 + 1) * 8, tb * 128:(tb + 1) * 128],
                                                                    in_=pT[:, :, :], func=AF.Copy),
                                  reads=[b_pT], writes=[b_x1T[tb]])
                        else:
                            kb.op(dve, lambda: nc.vector.tensor_copy(out=x1Tl[:, k8 * 8:(k8 + 1) * 8, :], in_=pT[:, :, :]),
                                  reads=[b_pT], writes=[b_x1Tl])
                pr, bpr = psb[6], b_ps[6]
                for k in range(16):
                    hi_k = x1Tb[:, k, tb * 128:(tb + 1) * 128]
                    kb.op(pe, lambda: nc.tensor.matmul(pr[:, 0:36], lhsT=hi_k, rhs=wrh[:, k * 36:(k + 1) * 36],
                                                       start=(k == 0), stop=False), reads=[b_x1T[tb], b_wrh], writes=[bpr])
                    kb.op(pe, lambda: nc.tensor.matmul(pr[:, 0:36], lhsT=hi_k, rhs=wrl[:, k * 36:(k + 1) * 36],
                                                       start=False, stop=False), reads=[b_x1T[tb], b_wrl], writes=[bpr])
                    kb.op(pe, lambda: nc.tensor.matmul(pr[:, 0:36], lhsT=x1Tl[:, k, :], rhs=wrh[:, k * 36:(k + 1) * 36],
                                                       start=False, stop=(k == 15)), reads=[b_x1Tl, b_wrh], writes=[bpr])
                if 'B4' in SKIP:
                    continue
                V = nc.vector
                R = lambda a, b=None: rt[:, a:(b if b is not None else a + 1)]
                kb.op(dve, lambda: V.tensor_tensor(out=lg[:], in0=pr[:, 0:36], in1=br[:], op=ALU.add), reads=[bpr, b_br], writes=[b_lg])
                seq = [
                    lambda: V.reduce_max(out=R(0), in_=lg[:, 0:4], axis=AX.X),
                    lambda: V.tensor_scalar(out=R(4, 8), in0=lg[:, 0:4], scalar1=R(0), scalar2=None, op0=ALU.is_equal),
                    lambda: V.tensor_scalar(out=R(8, 12), in0=lg[:, 0:4], scalar1=R(0), scalar2=None, op0=ALU.subtract),
                ]
                for f in seq:
                    kb.op(dve, f, reads=[b_lg, b_rt], writes=[b_rt])
                kb.op(act, lambda: nc.scalar.activation(out=R(8, 12), in_=R(8, 12), func=AF.Exp), reads=[b_rt], writes=[b_rt])
                seq = [
                    lambda: V.reduce_sum(out=R(1), in_=R(8, 12), axis=AX.X),
                    lambda: V.reciprocal(out=R(2), in_=R(1)),
                    lambda: V.tensor_scalar(out=R(12, 16), in0=R(4, 8), scalar1=-1.0, scalar2=1e30, op0=ALU.add, op1=ALU.mult),
                ]
                for f in seq:
                    kb.op(dve, f, reads=[b_rt], writes=[b_rt])
                for g in range(4):
                    kb.op(dve, lambda g=g: V.tensor_scalar(out=lem[:, g * 8:(g + 1) * 8], in0=lg[:, 4 + g * 8:12 + g * 8],
                                                           scalar1=R(12 + g), scalar2=None, op0=ALU.add),
                          reads=[b_lg, b_rt], writes=[b_lem])
                kb.op(dve, lambda: V.reduce_max(out=R(16), in_=lem[:], axis=AX.X), reads=[b_lem, b_rt], writes=[b_rt])
                kb.op(dve, lambda: V.tensor_scalar(out=oh1[:], in0=lem[:], scalar1=R(16), scalar2=None, op0=ALU.is_equal),
                      reads=[b_lem, b_rt], writes=[b_oh1])
                kb.op(dve, lambda: V.scalar_tensor_tensor(out=lem2[:], in0=oh1[:], scalar=-1e30, in1=lem[:], op0=ALU.mult, op1=ALU.add),
                      reads=[b_oh1, b_lem], writes=[b_lem2])
                kb.op(dve, lambda: V.reduce_max(out=R(17), in_=lem2[:], axis=AX.X), reads=[b_lem2, b_rt], writes=[b_rt])
                kb.op(dve, lambda: V.tensor_scalar(out=oh2[:], in0=lem2[:], scalar1=R(17), scalar2=None, op0=ALU.is_equal),
                      reads=[b_lem2, b_rt], writes=[b_oh2])
                kb.op(dve, lambda: V.tensor_tensor(out=R(18), in0=R(17), in1=R(16), op=ALU.subtract), reads=[b_rt], writes=[b_rt])
                kb.op(act, lambda: nc.scalar.activation(out=R(19), in_=R(18), func=AF.Exp), reads=[b_rt], writes=[b_rt])
                seq = [
                    lambda: V.tensor_scalar(out=R(20), in0=R(19), scalar1=1.0, scalar2=None, op0=ALU.add),
                    lambda: V.reciprocal(out=R(21), in_=R(20)),
                    lambda: V.tensor_tensor(out=R(22), in0=R(19), in1=R(21), op=ALU.mult),
                    lambda: V.tensor_tensor(out=R(23), in0=R(21), in1=R(2), op=ALU.mult),
                    lambda: V.tensor_tensor(out=R(24), in0=R(22), in1=R(2), op=ALU.mult),
                ]
                for f in seq:
                    kb.op(dve, f, reads=[b_rt], writes=[b_rt])
                cf = coef[:, tb * 32:(tb + 1) * 32]
                kb.op(dve, lambda: V.tensor_scalar(out=cf, in0=oh1[:], scalar1=R(23), scalar2=None, op0=ALU.mult),
                      reads=[b_oh1, b_rt], writes=[b_coef])
                kb.op(dve, lambda: V.scalar_tensor_tensor(out=cf, in0=oh2[:], scalar=R(24), in1=cf, op0=ALU.mult, op1=ALU.add),
                      reads=[b_oh2, b_rt, b_coef], writes=[b_coef])
            kb.barrier()
            kb.es = es
        yacc = kb.sb("yacc", [128, NT * D], F32)
        esM = ExitStack()
        kb.es = esM
        wgb = [kb.sb("wgb%d" % i, [128, 8192], BF16) for i in range(2)]
        wub = [kb.sb("wub%d" % i, [128, 8192], BF16) for i in range(2)]
        wdb = kb.sb("wdb", [128, 8192], BF16)
        hT = kb.sb("hT", [128, 4 * TT], BF16)
        sgl = [kb.sb("sgl%d" % i, [128, 512], F32) for i in range(2)]
        b_wg = [Buf(), Buf()]
        b_wu = [Buf(), Buf()]
        b_wd, b_hT, b_yacc = Buf(), Buf(), Buf()
        b_sgl = [Buf(), Buf()]
        b_ya = [Buf() for _ in range(NT)]

        def load_gu(e):
            for q in range(4):
                kb.dma(pool, wgb[e % 2][:, q * 2048:(q + 1) * 2048], wg_d[e][:, q * 2048:(q + 1) * 2048], writes=[b_wg[e % 2]])
            for q in range(4):
                kb.dma(pool, wub[e % 2][:, q * 2048:(q + 1) * 2048], wu_d[e][:, q * 2048:(q + 1) * 2048], writes=[b_wu[e % 2]])

        def load_d(e):
            for q in range(4):
                kb.dma(pool, wdb[:, q * 2048:(q + 1) * 2048], wd_d[e][:, q * 2048:(q + 1) * 2048], writes=[b_wd])

        load_gu(0)
        it = 0
        for e in range(32 if 'M' not in SKIP else 0):
            load_d(e)
            if e + 1 < 32:
                load_gu(e + 1)
            G, U = wgb[e % 2], wub[e % 2]
            for hf in range(NH):
                for j in range(4):
                    st = 2 * (it % 2)
                    it += 1
                    pg, pu = psb[st], psb[st + 1]
                    bpg, bpu = b_ps[st], b_ps[st + 1]
                    for k in range(16):
                        kb.op(pe, lambda k=k: nc.tensor.matmul(pg[:, :], lhsT=G[:, k * 512 + j * 128:k * 512 + (j + 1) * 128],
                                                               rhs=x1Tb[:, k, hf * 512:(hf + 1) * 512],
                                                               start=(k == 0), stop=(k == 15)),
                              reads=[b_wg[e % 2]] + b_x1T, writes=[bpg])
                    for k in range(16):
                        kb.op(pe, lambda k=k: nc.tensor.matmul(pu[:, :], lhsT=U[:, k * 512 + j * 128:k * 512 + (j + 1) * 128],
                                                               rhs=x1Tb[:, k, hf * 512:(hf + 1) * 512],
                                                               start=(k == 0), stop=(k == 15)),
                              reads=[b_wu[e % 2]] + b_x1T, writes=[bpu])
                    sg = sgl[it % 2]
                    bsg = b_sgl[it % 2]
                    kb.op(act, lambda: nc.scalar.activation(out=sg[:], in_=pg[:, :], func=AF.Silu), reads=[bpg], writes=[bsg])
                    kb.op(dve, lambda: nc.vector.tensor_tensor(out=hT[:, j * TT + hf * 512:j * TT + (hf + 1) * 512],
                                                               in0=sg[:], in1=pu[:, :], op=ALU.mult),
                          reads=[bsg, bpu], writes=[b_hT])
            for tb in range(NT):
                for n in range(4):
                    pd, bpd = psb[4 + (tb * 4 + n) % 3], b_ps[4 + (tb * 4 + n) % 3]
                    for j in range(4):
                        kb.op(pe, lambda j=j: nc.tensor.matmul(pd[:, :], lhsT=hT[:, j * TT + tb * 128:j * TT + (tb + 1) * 128],
                                                               rhs=wdb[:, j * 2048 + n * 512:j * 2048 + (n + 1) * 512],
                                                               start=(j == 0), stop=(j == 3)),
                              reads=[b_hT, b_wd], writes=[bpd])
                    ya = yacc[:, tb * D + n * 512:tb * D + (n + 1) * 512]
                    csc = coef[:, tb * 32 + e:tb * 32 + e + 1]
                    if e == 0:
                        kb.op(dve, lambda: nc.vector.tensor_scalar(out=ya, in0=pd[:, :], scalar1=csc, scalar2=None, op0=ALU.mult),
                              reads=[bpd, b_coef], writes=[b_ya[tb]])
                    else:
                        kb.op(dve, lambda: nc.vector.scalar_tensor_tensor(out=ya, in0=pd[:, :], scalar=csc, in1=ya,
                                                                          op0=ALU.mult, op1=ALU.add),
                              reads=[bpd, b_coef, b_ya[tb]], writes=[b_ya[tb]])
        kb.barrier()
        esM.close()
        kb.es = es
        lnp2 = kb.sb("lnp2", [128, 2 * D], F32)
        kb.dma(sp, lnp2[:], ln_d[:, 2 * D:4 * D], writes=[b_lnp2])
        x1r = [kb.sb("x1r%d" % i, [128, D], F32) for i in range(2)]
        b_x1r = [Buf(), Buf()]
        y2 = kb.sb("y2", [128, D], F32)
        o2 = [kb.sb("o2_%d" % i, [128, D], F32) for i in range(2)]
        stats2 = kb.sb("stats2", [128, 24], F32)
        mv2 = kb.sb("mv2", [128, 2], F32)
        b_y2, b_stats2, b_mv2 = Buf(), Buf(), Buf()
        b_o2 = [Buf(), Buf()]
        for tb in range(NT):
            r = tb % 2
            kb.dma(sp, x1r[r][:], x1_scr[tb * 128:(tb + 1) * 128, :], reads=[b_scr[tb]], writes=[b_x1r[r]])
            for n in range(4):
                kb.op(dve, lambda n=n: nc.vector.scalar_tensor_tensor(out=y2[:, n * 512:(n + 1) * 512],
                                                                     in0=x1r[r][:, n * 512:(n + 1) * 512], scalar=ALPHA,
                                                                     in1=yacc[:, tb * D + n * 512:tb * D + (n + 1) * 512],
                                                                     op0=ALU.mult, op1=ALU.add),
                      reads=[b_x1r[r], b_ya[tb]], writes=[b_y2])
                kb.op(dve, lambda n=n: nc.vector.bn_stats(out=stats2[:, n * 6:(n + 1) * 6], in_=y2[:, n * 512:(n + 1) * 512]),
                      reads=[b_y2], writes=[b_stats2])
            _ln_tail(nc, kb, y2, b_y2, stats2, b_stats2, mv2, b_mv2, o2[r], b_o2[r], lnp2, b_lnp2)
            tok = kb.dma(sp, out_d[tb * 128:(tb + 1) * 128, :], o2[r][:], reads=[b_o2[r]])
            kb.out_toks.append(tok)
        for tok in kb.out_toks:
            sp.need(tok)
    return nc


def _ln_tail(nc, kb, yv, b_yv, stats, b_stats, mv, b_mv, outt, b_out, lnp, b_lnp):
    dve, act = kb.dve, kb.act
    V = nc.vector
    kb.op(dve, lambda: V.bn_aggr(out=mv[:], in_=stats[:]), reads=[b_stats], writes=[b_mv])
    kb.op(dve, lambda: V.tensor_scalar(out=mv[:, 1:2], in0=mv[:, 1:2], scalar1=LN_EPS, scalar2=None, op0=ALU.add),
          reads=[b_mv], writes=[b_mv])
    kb.op(act, lambda: nc.scalar.activation(out=mv[:, 1:2], in_=mv[:, 1:2], func=AF.Sqrt), reads=[b_mv], writes=[b_mv])
    kb.op(dve, lambda: V.reciprocal(out=mv[:, 1:2], in_=mv[:, 1:2]), reads=[b_mv], writes=[b_mv])
    kb.op(dve, lambda: V.tensor_scalar(out=outt[:], in0=yv[:], scalar1=mv[:, 0:1], scalar2=mv[:, 1:2],
                                       op0=ALU.subtract, op1=ALU.mult), reads=[b_yv, b_mv], writes=[b_out])
    kb.op(dve, lambda: V.tensor_tensor(out=outt[:], in0=outt[:], in1=lnp[:, 0:D], op=ALU.mult), reads=[b_out, b_lnp], writes=[b_out])
    kb.op(dve, lambda: V.tensor_tensor(out=outt[:], in0=outt[:], in1=lnp[:, D:2 * D], op=ALU.add), reads=[b_out, b_lnp], writes=[b_out])


def _rep(v, n=128):
    v = np.asarray(v, np.float32).reshape(1, -1)
    return np.ascontiguousarray(np.repeat(v, n, 0))


def l2_weights(inp):
    w = np.asarray(inp["w_in"], np.float32).reshape(D, -1)
    wgate = w[:, 6152:6152 + 4096]
    wpd = np.asarray(inp["w_proj_diff"], np.float32).reshape(1024, D)
    wpf = np.asarray(inp["w_proj_fox"], np.float32).reshape(1024, D)
    slabs = []
    for j in range(16):
        cols = slice(j * 128, (j + 1) * 128)
        gd = wgate[:, j * 128:(j + 1) * 128].reshape(16, 128, 128)
        gf = wgate[:, 2048 + j * 128:2048 + (j + 1) * 128].reshape(16, 128, 128)
        pd = wpd[:, cols].reshape(8, 128, 128)
        pf = wpf[:, cols].reshape(8, 128, 128)
        sl = np.concatenate([gd, gf, pd, pf], 0)
        slabs.append(sl.transpose(1, 0, 2).reshape(128, 48 * 128))
    wmix = np.ascontiguousarray(np.stack(slabs, 0))
    wout = _pk(np.asarray(inp["w_out"], np.float32).reshape(D, D))
    lnp = np.concatenate([_rep(inp["ln1_g"]), _rep(inp["ln1_b"]), _rep(inp["ln2_g"]), _rep(inp["ln2_b"])], 1)
    wrg = np.asarray(inp["w_router_group"], np.float32).reshape(D, 4)
    wre = np.asarray(inp["w_router_expert"], np.float32).reshape(4, D, 8).transpose(1, 0, 2).reshape(D, 32)
    wr = _pk(np.concatenate([wrg, wre], 1))
    br = _rep(np.concatenate([np.asarray(inp["b_router_group"], np.float32).reshape(-1),
                              np.asarray(inp["b_router_expert"], np.float32).reshape(-1)]))
    wg = np.asarray(inp["w_gate"], np.float32).reshape(32, 16, 128, 512).transpose(0, 2, 1, 3).reshape(32, 128, 8192)
    wu = np.asarray(inp["w_up"], np.float32).reshape(32, 16, 128, 512).transpose(0, 2, 1, 3).reshape(32, 128, 8192)
    wd = np.asarray(inp["w_down"], np.float32).reshape(32, 4, 128, D).transpose(0, 2, 1, 3).reshape(32, 128, 8192)
    lamv = np.concatenate([_rep(inp["lam_q1"]), _rep(inp["lam_k1"]), _rep(inp["lam_q2"]), _rep(inp["lam_k2"])], 1)
    gcol = np.ascontiguousarray(np.asarray(inp["diff_norm_g"], np.float32).reshape(8, 128).T)
    return {"lamv": lamv, "gcol": gcol, "wmix": wmix, "wout": wout, "lnp": np.ascontiguousarray(lnp), "wr": wr, "br": br,
            "wg": np.ascontiguousarray(wg), "wu": np.ascontiguousarray(wu), "wd": np.ascontiguousarray(wd),
            "ident": np.eye(128, dtype=np.float32)}


def _fm(a, TT):
    nf = a.shape[1] // 128
    return np.ascontiguousarray(a.reshape(TT, nf, 128).transpose(2, 1, 0).reshape(128, nf * TT))


def run_l2(inp, x2, o_f, o_d, TT, ncore):
    wts = l2_weights(inp)
    in_maps = []
    for c in range(ncore):
        tsl = slice(c * TT, (c + 1) * TT)
        xc = np.ascontiguousarray(x2[tsl])
        o1 = np.concatenate([o_d[2 * h][tsl] for h in range(4)], 1)
        o2 = np.concatenate([o_d[2 * h + 1][tsl] for h in range(4)], 1)
        of = np.concatenate([o_f[h][tsl] for h in range(8)], 1)
        m = dict(wts)
        m.update({"xT": _fm(xc, TT), "x": xc, "o1T": _fm(o1, TT), "o2T": _fm(o2, TT), "ofT": _fm(of, TT)})
        in_maps.append(m)
    nc = build_l2(TT)
    res = run_bass_kernel_spmd(nc, in_maps, core_ids=list(range(ncore)))
    return np.concatenate([r["out"] for r in res.results], 0)


def kernel(**inp):
    x = np.asarray(inp["x"], np.float32)
    o_f, o_d = run_l1(inp["x"], inp["w_in"], inp["b_forget"])
    out = run_l2(inp, x.reshape(S, D), o_f, o_d, S // NCORE, NCORE)
    return out.reshape(1, S, D).astype(np.float32)
```

```python
import math
from contextlib import ExitStack

import numpy as np
import concourse.bass as bass
import concourse.mybir as mybir
from concourse.bass_utils import run_bass_kernel_spmd

F32 = mybir.dt.float32
BF16 = mybir.dt.bfloat16
I32 = mybir.dt.int32
AF = mybir.ActivationFunctionType
ALU = mybir.AluOpType
AX = mybir.AxisListType

D = 2048
S = 8192
NB = S // 128
NQ = S // 512
NCORE = 8
HD = 128
SCALE = HD ** -0.5
NEG = -30000.0
LN_EPS = 1e-5
ALPHA = 2.0 ** 0.25
LAM_INIT = 0.2


class Buf:
    __slots__ = ("w", "r")

    def __init__(self):
        self.w = None
        self.r = {}


class Eng:
    def __init__(self, nc, eng, name, es, strict=True):
        self.eng = eng
        self.name = name
        self.sem = es.enter_context(nc.semaphore("sem_" + name))
        self.cnt = 0
        self.waited = {}
        self.strict = strict

    def need(self, tok):
        if tok is None:
            return
        sem, val, owner = tok
        if owner is self and not self.strict:
            return
        key = id(sem)
        if self.waited.get(key, 0) >= val:
            return
        self.eng.wait_ge(sem, val)
        self.waited[key] = val


class KB:
    def __init__(self, nc, es, ndma=40):
        self.nc = nc
        self.es = es
        self.pe = Eng(nc, nc.tensor, "pe", es, strict=False)
        self.act = Eng(nc, nc.scalar, "act", es)
        self.dve = Eng(nc, nc.vector, "dve", es)
        self.pool = Eng(nc, nc.gpsimd, "pool", es)
        self.sp = Eng(nc, nc.sync, "sp", es)
        self.dsem = [es.enter_context(nc.semaphore("dma%d" % i)) for i in range(ndma)]
        self.dcnt = [0] * ndma
        self.di = {"hw": 0, "sw": ndma // 2}
        self.out_toks = []

    def _pre(self, E, reads, writes):
        for b in reads:
            E.need(b.w)
        for b in writes:
            E.need(b.w)
            for t in b.r.values():
                E.need(t)

    def _post(self, tok, reads, writes):
        for b in reads:
            k = id(tok[0])
            old = b.r.get(k)
            if old is None or old[1] < tok[1]:
                b.r[k] = tok
        for b in writes:
            b.w = tok
            b.r = {}

    def op(self, E, fn, reads=(), writes=()):
        self._pre(E, reads, writes)
        inst = fn()
        E.cnt += 1
        inst.then_inc(E.sem, 1)
        tok = (E.sem, E.cnt, E)
        self._post(tok, reads, writes)
        return tok

    def dma(self, E, out, in_, reads=(), writes=(), **kw):
        self._pre(E, reads, writes)
        half = len(self.dsem) // 2
        if E is self.pool:
            i = self.di["sw"]
            self.di["sw"] = half + (i + 1 - half) % half
        else:
            i = self.di["hw"]
            self.di["hw"] = (i + 1) % half
        sem = self.dsem[i]
        if self.dcnt[i] > 0:
            E.need((sem, self.dcnt[i], None))
        inst = E.eng.dma_start(out=out, in_=in_, **kw)
        self.dcnt[i] += 16
        inst.then_inc(sem, 16)
        tok = (sem, self.dcnt[i], None)
        self._post(tok, reads, writes)
        return tok

    def barrier(self):
        engs = [self.pe, self.act, self.dve, self.pool, self.sp]
        for E in engs:
            for Fe in engs:
                if Fe is not E and Fe.cnt > 0:
                    E.need((Fe.sem, Fe.cnt, Fe))
            for i, c in enumerate(self.dcnt):
                if c > 0:
                    E.need((self.dsem[i], c, None))

    def sb(self, name, shape, dt):
        return self.es.enter_context(self.nc.sbuf_tensor("s_" + name, shape, dt))

    def ps(self, name, shape, dt=F32):
        return self.es.enter_context(self.nc.psum_tensor("p_" + name, shape, dt))


def build_l1():
    nc = bass.Bass("TRN2", target_bir_lowering=False)
    xTb = nc.dram_tensor("xTb", [NB, 128, 16, 128], F32, kind="ExternalInput").ap()
    wq_d = nc.dram_tensor("wq", [128, 16 * 768], F32, kind="ExternalInput").ap()
    wv_d = nc.dram_tensor("wv", [128, 16 * 385], F32, kind="ExternalInput").ap()
    cs_d = nc.dram_tensor("cs", [NB, 128, 256], F32, kind="ExternalInput").ap()
    negb_d = nc.dram_tensor("negb", [128, 1], F32, kind="ExternalInput").ap()
    cmask_d = nc.dram_tensor("cmask", [128, 4 * 512], F32, kind="ExternalInput").ap()
    cmat_d = nc.dram_tensor("cmat", [128, 4 * 128], F32, kind="ExternalInput").ap()
    of_d = nc.dram_tensor("o_f", [S, 128], F32, kind="ExternalOutput").ap()
    od_d = nc.dram_tensor("o_d", [S, 256], F32, kind="ExternalOutput").ap()

    with ExitStack() as es:
        kb = KB(nc, es)
        pe, act, dve, pool, sp = kb.pe, kb.act, kb.dve, kb.pool, kb.sp

        fqT = kb.sb("fqT", [128, S], BF16)
        fkT = kb.sb("fkT", [128, S], BF16)
        dqT = kb.sb("dqT", [128, S], BF16)
        dkT = kb.sb("dkT", [128, S], BF16)
        Vf = kb.sb("Vf", [128, NB, 129], BF16)
        Vd = kb.sb("Vd", [128, NB, 257], BF16)
        zcol = kb.sb("zcol", [128, NB], F32)
        cmat = kb.sb("cmat", [128, 512], F32)
        identb = kb.sb("identb", [128, 128], BF16)
        maskb = kb.sb("maskb", [128, 4 * 512], BF16)
        negb = kb.sb("negb", [128, 1], F32)
        ones3 = kb.sb("ones3", [128, 128], BF16)
        b_fq = [Buf() for _ in range(NB)]
        b_fk = [Buf() for _ in range(NB)]
        b_dq = [Buf() for _ in range(NB)]
        b_dk = [Buf() for _ in range(NB)]
        b_vf = [Buf() for _ in range(NB)]
        b_vd = [Buf() for _ in range(NB)]
        b_z = Buf()
        b_cmat, b_identb, b_maskb, b_negb, b_ones3 = Buf(), Buf(), Buf(), Buf(), Buf()

        psb = [kb.ps("ps%d" % i, [128, 512]) for i in range(8)]
        b_ps = [Buf() for _ in range(8)]

        kb.dma(sp, cmat[:], cmat_d[:, :], writes=[b_cmat])
        kb.dma(sp, negb[:], negb_d[:, :], writes=[b_negb])
        kb.dma(pool, identb[:], cmat_d[:, 0:128], writes=[b_identb])
        for j in range(4):
            kb.dma(pool, maskb[:, j * 512:(j + 1) * 512], cmask_d[:, j * 512:(j + 1) * 512], writes=[b_maskb])
        kb.op(dve, lambda: nc.vector.memset(ones3[:], 0.0), writes=[b_ones3])
        kb.op(dve, lambda: nc.vector.memset(ones3[0:3, :], 1.0), writes=[b_ones3])
        kb.op(dve, lambda: nc.vector.memset(Vf[:, :, 128:129], 1.0), writes=b_vf)
        kb.op(dve, lambda: nc.vector.memset(Vd[:, :, 256:257], 1.0), writes=b_vd)

        with ExitStack() as es1:
            kb1 = kb
            old_es = kb.es
            kb.es = es1
            wq = kb.sb("wq", [128, 16 * 768], BF16)
            wv = kb.sb("wv", [128, 16 * 385], BF16)
            xs = kb.sb("xs", [128, 16 * 128], F32)
            xb = [kb.sb("xb%d" % i, [128, 16 * 128], BF16) for i in range(2)]
            cst = [kb.sb("cst%d" % i, [128, 256], F32) for i in range(2)]
            t1 = kb.sb("t1", [128, 128], F32)
            t2 = kb.sb("t2", [128, 128], F32)
            b_wq, b_wv, b_xs = Buf(), Buf(), Buf()
            b_xb = [Buf(), Buf()]
            b_cst = [Buf(), Buf()]
            b_t1, b_t2 = Buf(), Buf()
            for k in range(16):
                kb.dma(pool, wq[:, k * 768:(k + 1) * 768], wq_d[:, k * 768:(k + 1) * 768], writes=[b_wq])
            for k in range(16):
                kb.dma(pool, wv[:, k * 385:(k + 1) * 385], wv_d[:, k * 385:(k + 1) * 385], writes=[b_wv])

            def load_block(tb):
                kb.dma(sp, xs[:], xTb[tb].rearrange("p k t -> p (k t)"), writes=[b_xs])
                kb.dma(sp, cst[tb % 2][:], cs_d[tb], writes=[b_cst[tb % 2]])
                kb.op(pool, lambda: nc.gpsimd.tensor_copy(out=xb[tb % 2][:], in_=xs[:]),
                      reads=[b_xs], writes=[b_xb[tb % 2]])

            load_block(0)
            for tb in range(NB):
                if tb + 1 < NB:
                    load_block(tb + 1)
                s = tb % 2
                X = xb[s]
                bA, bB, bC = psb[3 * s], psb[3 * s + 1], psb[3 * s + 2]
                BA, BB, BC = b_ps[3 * s], b_ps[3 * s + 1], b_ps[3 * s + 2]
                for m in range(6):
                    bank, bb = (bA, BA) if m < 4 else (bB, BB)
                    col = (m % 4) * 128
                    for k in range(16):
                        kb.op(pe, lambda k=k, m=m, bank=bank, col=col: nc.tensor.matmul(
                            bank[:, col:col + 128], lhsT=wq[:, k * 768 + m * 128:k * 768 + (m + 1) * 128],
                            rhs=X[:, k * 128:(k + 1) * 128], start=(k == 0), stop=(k == 15)),
                            reads=[b_wq, b_xb[s]], writes=[bb])
                for k in range(16):
                    kb.op(pe, lambda k=k: nc.tensor.matmul(
                        bC[:, 0:385], lhsT=X[:, k * 128:(k + 1) * 128], rhs=wv[:, k * 385:(k + 1) * 385],
                        start=(k == 0), stop=(k == 15)), reads=[b_wv, b_xb[s]], writes=[BC])
                tsl = slice(tb * 128, (tb + 1) * 128)
                C0 = cst[s][:, 0:128]
                C1 = cst[s][:, 128:256]
                kb.op(dve, lambda: nc.vector.scalar_tensor_tensor(out=t1[:], in0=bA[:, 0:128], scalar=SCALE, in1=C0,
                                                                  op0=ALU.mult, op1=ALU.mult),
                      reads=[BA, b_cst[s]], writes=[b_t1])
                kb.op(dve, lambda: nc.vector.scalar_tensor_tensor(out=t2[:], in0=bA[:, 128:256], scalar=SCALE, in1=C1,
                                                                  op0=ALU.mult, op1=ALU.mult),
                      reads=[BA, b_cst[s]], writes=[b_t2])
                kb.op(dve, lambda: nc.vector.tensor_tensor(out=dqT[:, tsl], in0=t1[:], in1=t2[:], op=ALU.add),
                      reads=[b_t1, b_t2], writes=[b_dq[tb]])
                kb.op(dve, lambda: nc.vector.tensor_tensor(out=t1[:], in0=bA[:, 256:384], in1=C0, op=ALU.mult),
                      reads=[BA, b_cst[s]], writes=[b_t1])
                kb.op(dve, lambda: nc.vector.tensor_tensor(out=t2[:], in0=bA[:, 384:512], in1=C1, op=ALU.mult),
                      reads=[BA, b_cst[s]], writes=[b_t2])
                kb.op(dve, lambda: nc.vector.tensor_tensor(out=dkT[:, tsl], in0=t1[:], in1=t2[:], op=ALU.add),
                      reads=[b_t1, b_t2], writes=[b_dk[tb]])
                kb.op(act, lambda: nc.scalar.activation(out=fqT[:, tsl], in_=bB[:, 0:128], func=AF.Copy, scale=SCALE),
                      reads=[BB], writes=[b_fq[tb]])
                kb.op(act, lambda: nc.scalar.activation(out=fkT[:, tsl], in_=bB[:, 128:256], func=AF.Copy),
                      reads=[BB], writes=[b_fk[tb]])
                kb.op(act, lambda: nc.scalar.activation(out=Vd[:, tb, 0:256], in_=bC[:, 0:256], func=AF.Copy),
                      reads=[BC], writes=[b_vd[tb]])
                kb.op(act, lambda: nc.scalar.activation(out=Vf[:, tb, 0:128], in_=bC[:, 256:384], func=AF.Copy),
                      reads=[BC], writes=[b_vf[tb]])
                kb.op(act, lambda: nc.scalar.activation(out=zcol[:, tb:tb + 1], in_=bC[:, 384:385], func=AF.Copy),
                      reads=[BC], writes=[b_z])
            kb.barrier()
            kb.es = old_es

        cmatb = kb.sb("cmatb", [128, 512], BF16)
        onesb = kb.sb("onesb", [128, 128], BF16)
        b_cmatb, b_onesb = Buf(), Buf()
        kb.dma(pool, cmatb[:], cmat_d[:, :], writes=[b_cmatb])
        kb.op(dve, lambda: nc.vector.memset(onesb[:], 1.0), writes=[b_onesb])
        identc = cmatb[:, 0:128]
        trib = cmatb[:, 128:256]
        e0b = cmatb[:, 256:384]
        gselb = cmatb[:, 384:512]
        spc = kb.sb("spc", [128, NB], F32)
        ones64 = kb.sb("ones64", [128, NB], F32)
        tot = kb.sb("tot", [128, NB], F32)
        cinc = kb.sb("cinc", [128, NB], F32)
        ccol = kb.sb("ccol", [128, NB], F32)
        crefb = kb.sb("crefb", [128, NQ], F32)
        biasK = kb.sb("biasK", [128, NQ * NB], F32)
        cT = kb.sb("cT", [NB, 128], F32)
        crefT = kb.sb("crefT", [NB, 1], F32)
        crel = kb.sb("crel", [NB, 128], F32)
        crel3 = kb.sb("crel3", [128, S], BF16)
        b_spc, b_ones64, b_tot, b_cinc, b_ccol, b_crefb, b_biasK = (Buf() for _ in range(7))
        b_cT, b_crefT, b_crel, b_crel3 = (Buf() for _ in range(4))
        st1 = kb.sb("st1", [128, 128], F32)
        st2 = kb.sb("st2", [128, 128], F32)
        b_st1, b_st2 = Buf(), Buf()

        def split3(name, src, b_src, P_, N_):
            parts = [kb.sb("%s_p%d" % (name, i), [P_, N_], BF16) for i in range(3)]
            bp = [Buf() for _ in range(3)]
            V = nc.vector
            kb.op(dve, lambda: V.tensor_copy(out=parts[0][:], in_=src), reads=[b_src], writes=[bp[0]])
            kb.op(dve, lambda: V.tensor_tensor(out=st1[0:P_, 0:N_], in0=src, in1=parts[0][:], op=ALU.subtract),
                  reads=[b_src, bp[0]], writes=[b_st1])
            kb.op(dve, lambda: V.tensor_copy(out=parts[1][:], in_=st1[0:P_, 0:N_]), reads=[b_st1], writes=[bp[1]])
            kb.op(dve, lambda: V.tensor_tensor(out=st2[0:P_, 0:N_], in0=st1[0:P_, 0:N_], in1=parts[1][:], op=ALU.subtract),
                  reads=[b_st1, bp[1]], writes=[b_st2])
            kb.op(dve, lambda: V.tensor_copy(out=parts[2][:], in_=st2[0:P_, 0:N_]), reads=[b_st2], writes=[bp[2]])
            return parts, bp

        def mm3(out_ap, bout, lhs_fn, rhs_fn, extra_reads):
            for i in range(3):
                kb.op(pe, lambda i=i: nc.tensor.matmul(out_ap, lhsT=lhs_fn(i), rhs=rhs_fn(i), start=(i == 0), stop=(i == 2)),
                      reads=extra_reads(i), writes=[bout])

        kb.op(dve, lambda: nc.vector.memset(ones64[:], 1.0), writes=[b_ones64])
        kb.op(dve, lambda: nc.vector.tensor_scalar(out=zcol[:], in0=zcol[:], scalar1=negb[:, 0:1], scalar2=None,
                                                   op0=ALU.add), reads=[b_z, b_negb], writes=[b_z])
        kb.op(act, lambda: nc.scalar.activation(out=spc[:], in_=zcol[:], func=AF.Exp, scale=-1.0),
              reads=[b_z], writes=[b_spc])
        kb.op(act, lambda: nc.scalar.activation(out=spc[:], in_=spc[:], func=AF.Ln, bias=1.0, scale=1.0),
              reads=[b_spc], writes=[b_spc])
        P0, P1 = psb[0], psb[1]
        B0, B1 = b_ps[0], b_ps[1]
        spp, bspp = split3("spp", spc[:], b_spc, 128, NB)
        mm3(P0[:, 0:NB], B0, lambda i: trib, lambda i: spp[i][:], lambda i: [b_cmatb, bspp[i]])
        mm3(P1[:, 0:NB], B1, lambda i: onesb[:], lambda i: spp[i][:], lambda i: [b_onesb, bspp[i]])
        kb.op(dve, lambda: nc.vector.tensor_copy(out=tot[:], in_=P1[:, 0:NB]), reads=[B1], writes=[b_tot])
        kb.op(dve, lambda: nc.vector.tensor_tensor_scan(out=cinc[:], data0=ones64[:], data1=tot[:], initial=0.0,
                                                        op0=ALU.mult, op1=ALU.add),
              reads=[b_ones64, b_tot], writes=[b_cinc])
        kb.op(dve, lambda: nc.vector.tensor_tensor(out=cinc[:], in0=cinc[:], in1=tot[:], op=ALU.subtract),
              reads=[b_cinc, b_tot], writes=[b_cinc])
        kb.op(dve, lambda: nc.vector.tensor_tensor(out=cinc[:], in0=cinc[:], in1=P0[:, 0:NB], op=ALU.add),
              reads=[b_cinc, B0], writes=[b_cinc])
        kb.op(dve, lambda: nc.vector.tensor_scalar(out=ccol[:], in0=cinc[:], scalar1=-1.0, scalar2=None, op0=ALU.mult),
              reads=[b_cinc], writes=[b_ccol])
        ccp, bccp = split3("ccp", ccol[:], b_ccol, 128, NB)
        mm3(P0[:, 64:64 + NQ], B0, lambda i: e0b, lambda i: ccp[i][:, 0:NB:4], lambda i: [b_cmatb, bccp[i]])
        kb.op(dve, lambda: nc.vector.tensor_copy(out=crefb[:], in_=P0[:, 64:64 + NQ]), reads=[B0], writes=[b_crefb])
        for qc in range(NQ):
            kb.op(dve, lambda qc=qc: nc.vector.tensor_scalar(
                out=biasK[:, qc * NB:(qc + 1) * NB], in0=ccol[:], scalar1=-1.0, scalar2=crefb[:, qc:qc + 1],
                op0=ALU.mult, op1=ALU.add), reads=[b_ccol, b_crefb], writes=[b_biasK])
        mm3(P1[0:NB, 128:256], B1, lambda i: ccp[i][:, 0:NB], lambda i: identc, lambda i: [b_cmatb, bccp[i]])
        kb.op(dve, lambda: nc.vector.tensor_copy(out=cT[:], in_=P1[0:NB, 128:256]), reads=[B1], writes=[b_cT])
        cTp, bcTp = split3("cTp", cT[:], b_cT, NB, 128)
        mm3(P1[0:NB, 256:257], B1, lambda i: gselb[0:NB, 0:NB], lambda i: cTp[i][:, 0:1], lambda i: [b_cmatb, bcTp[i]])
        kb.op(dve, lambda: nc.vector.tensor_copy(out=crefT[:], in_=P1[0:NB, 256:257]), reads=[B1], writes=[b_crefT])
        kb.op(dve, lambda: nc.vector.tensor_scalar(out=crel[:], in0=cT[:], scalar1=crefT[:, 0:1], scalar2=None,
                                                   op0=ALU.subtract), reads=[b_cT, b_crefT], writes=[b_crel])
        crp, bcrp = split3("crp", crel[:], b_crel, NB, 128)
        scr = nc.dram_tensor("scr_c", [3, S], BF16).ap()
        b_scr = Buf()
        for j in range(3):
            kb.dma(sp, scr[j].rearrange("(b p) -> b p", p=128), crp[j][:], reads=[bcrp[j]], writes=[b_scr])
        kb.op(pool, lambda: nc.gpsimd.memset(crel3[:], 0.0), writes=[b_crel3])
        kb.dma(sp, crel3[0:3, :], scr[:, :], reads=[b_scr], writes=[b_crel3])

        Pt = [kb.sb("Pt%d" % i, [128, 512], BF16) for i in range(3)]
        b_Pt = [Buf() for _ in range(3)]
        osb = [kb.sb("osb%d" % i, [128, 256], F32) for i in range(2)]
        b_osb = [Buf(), Buf()]
        rcp = [kb.sb("rcp%d" % i, [128, 1], F32) for i in range(2)]
        b_rcp = [Buf(), Buf()]
        ocnt = [0]

        def attn_pass(qT, bq, kT, bk, V, bv, dv, use_bias, out_d):
            tiles = [(qc, kblk) for qc in range(NQ) for kblk in range(4 * qc + 4)]
            nt = len(tiles)

            def emit_S(i):
                qc, kblk = tiles[i]
                bank, bb = psb[i % 2], b_ps[i % 2]
                diag = kblk >= 4 * qc
                qsl = slice(qc * 512, (qc + 1) * 512)
                kb.op(pe, lambda: nc.tensor.matmul(bank[:, :], lhsT=kT[:, kblk * 128:(kblk + 1) * 128], rhs=qT[:, qsl],
                                                   start=True, stop=not (use_bias or diag)),
                      reads=[bk[kblk]] + bq[4 * qc:4 * qc + 4], writes=[bb])
                if use_bias:
                    kb.op(pe, lambda: nc.tensor.matmul(bank[:, :], lhsT=ones3[:, :], rhs=crel3[:, qsl],
                                                       start=False, stop=not diag),
                          reads=[b_ones3, b_crel3], writes=[bb])
                if diag:
                    j = kblk - 4 * qc
                    kb.op(pe, lambda: nc.tensor.matmul(bank[:, :], lhsT=identb[:, :], rhs=maskb[:, j * 512:(j + 1) * 512],
                                                       start=False, stop=True),
                          reads=[b_identb, b_maskb], writes=[bb])

            def emit_exp(i):
                qc, kblk = tiles[i]
                bank, bb = psb[i % 2], b_ps[i % 2]
                if use_bias:
                    kb.op(act, lambda: nc.scalar.activation(out=Pt[i % 3][:], in_=bank[:, :], func=AF.Exp,
                                                            bias=biasK[:, qc * NB + kblk:qc * NB + kblk + 1], scale=1.0),
                          reads=[bb, b_biasK], writes=[b_Pt[i % 3]])
                else:
                    kb.op(act, lambda: nc.scalar.activation(out=Pt[i % 3][:], in_=bank[:, :], func=AF.Exp),
                          reads=[bb], writes=[b_Pt[i % 3]])

            def emit_PV(i):
                qc, kblk = tiles[i]
                for sub in range(4):
                    qb = 4 * qc + sub
                    if kblk > qb:
                        continue
                    ob, bob = psb[2 + sub], b_ps[2 + sub]
                    kb.op(pe, lambda: nc.tensor.matmul(ob[:, 0:dv + 1], lhsT=Pt[i % 3][:, sub * 128:(sub + 1) * 128],
                                                       rhs=V[:, kblk, 0:dv + 1], start=(kblk == 0), stop=(kblk == qb)),
                          reads=[b_Pt[i % 3], bv[kblk]], writes=[bob])
                    if kblk == qb:
                        o = ocnt[0] % 2
                        ocnt[0] += 1
                        kb.op(dve, lambda: nc.vector.reciprocal(out=rcp[o][:], in_=ob[:, dv:dv + 1]),
                              reads=[bob], writes=[b_rcp[o]])
                        kb.op(dve, lambda: nc.vector.tensor_scalar(out=osb[o][:, 0:dv], in0=ob[:, 0:dv],
                                                                   scalar1=rcp[o][:, 0:1], scalar2=None, op0=ALU.mult),
                              reads=[bob, b_rcp[o]], writes=[b_osb[o]])
                        tok = kb.dma(pool, out_d[qb * 128:(qb + 1) * 128, :], osb[o][:, 0:dv], reads=[b_osb[o]])
                        kb.out_toks.append(tok)

            emit_S(0)
            for i in range(nt):
                if i + 1 < nt:
                    emit_S(i + 1)
                emit_exp(i)
                emit_PV(i)

        attn_pass(fqT, b_fq, fkT, b_fk, Vf, b_vf, 128, True, of_d)
        attn_pass(dqT, b_dq, dkT, b_dk, Vd, b_vd, 256, False, od_d)
        for tok in kb.out_toks:
            pool.need(tok)
    return nc


def _rope_tables():
    inv_freq = (10000.0 ** (-np.arange(0, HD, 2, dtype=np.float32) / np.float32(HD))).astype(np.float32)
    ang = np.arange(S, dtype=np.float32)[:, None] * inv_freq[None, :]
    cos = np.cos(ang).astype(np.float32).T
    sin = np.sin(ang).astype(np.float32).T
    c0 = np.concatenate([cos, cos], 0)
    c1 = np.concatenate([-sin, sin], 0)
    cs = np.stack([c0, c1], 1)
    cs = cs.reshape(128, 2, NB, 128).transpose(2, 0, 1, 3).reshape(NB, 128, 256)
    return np.ascontiguousarray(cs)


def _l1_consts():
    k = np.arange(128)[:, None]
    q = np.arange(512)[None, :]
    cmask = np.concatenate([np.where(q - k - 128 * j >= 0, 0.0, NEG) for j in range(4)], 1).astype(np.float32)
    ident = np.eye(128, dtype=np.float32)
    tri = (np.arange(128)[:, None] <= np.arange(128)[None, :]).astype(np.float32)
    e0 = np.zeros((128, 128), np.float32)
    e0[0, :] = 1.0
    g = np.zeros((128, 128), np.float32)
    for b in range(64):
        g[4 * (b // 4), b] = 1.0
    cmat = np.concatenate([ident, tri, e0, g], 1)
    return cmask, np.ascontiguousarray(cmat)


def _pk(w):
    n = w.shape[1]
    return np.ascontiguousarray(w.reshape(16, 128, n).transpose(1, 0, 2).reshape(128, 16 * n))


def run_l1(x, w_in, b_forget):
    x2 = np.asarray(x, np.float32).reshape(S, D)
    w = np.asarray(w_in, np.float32).reshape(D, -1)
    xTb = np.ascontiguousarray(x2.reshape(NB, 128, 16, 128).transpose(0, 3, 2, 1))
    cs = _rope_tables()
    cmask, cmat = _l1_consts()
    sw = np.concatenate([np.arange(64, 128), np.arange(0, 64)])
    in_maps = []
    for c in range(NCORE):
        h = c // 2
        dq = w[:, c * 128:(c + 1) * 128]
        dk = w[:, 1024 + c * 128:1024 + (c + 1) * 128]
        dv = w[:, 2048 + h * 256:2048 + (h + 1) * 256]
        fq = w[:, 3072 + c * 128:3072 + (c + 1) * 128]
        fk = w[:, 4096 + c * 128:4096 + (c + 1) * 128]
        fv = w[:, 5120 + c * 128:5120 + (c + 1) * 128]
        fl = w[:, 6144 + c:6144 + c + 1]
        wq = np.concatenate([dq, dq[:, sw], dk, dk[:, sw], fq, fk], 1)
        wv = np.concatenate([dv, fv, fl], 1)
        bfc = np.asarray(b_forget, np.float32).reshape(-1)[c]
        in_maps.append({"xTb": xTb, "wq": _pk(wq), "wv": _pk(wv), "cs": cs,
                        "negb": np.full((128, 1), 1.0, np.float32) * bfc,
                        "cmask": cmask, "cmat": cmat})
    nc = build_l1()
    res = run_bass_kernel_spmd(nc, in_maps, core_ids=list(range(NCORE)))
    o_f = [r["o_f"] for r in res.results]
    o_d = [r["o_d"] for r in res.results]
    return o_f, o_d


SKIP = set()


def build_l2(TT):
    NT = TT // 128
    NH = TT // 512
    nc = bass.Bass("TRN2", target_bir_lowering=False)
    xT_d = nc.dram_tensor("xT", [128, 16 * TT], F32, kind="ExternalInput").ap()
    x_d = nc.dram_tensor("x", [TT, D], F32, kind="ExternalInput").ap()
    o1T_d = nc.dram_tensor("o1T", [128, 8 * TT], F32, kind="ExternalInput").ap()
    o2T_d = nc.dram_tensor("o2T", [128, 8 * TT], F32, kind="ExternalInput").ap()
    ofT_d = nc.dram_tensor("ofT", [128, 8 * TT], F32, kind="ExternalInput").ap()
    lamv_d = nc.dram_tensor("lamv", [128, 512], F32, kind="ExternalInput").ap()
    gcol_d = nc.dram_tensor("gcol", [128, 8], F32, kind="ExternalInput").ap()
    wmix_d = nc.dram_tensor("wmix", [16, 128, 6144], F32, kind="ExternalInput").ap()
    wout_d = nc.dram_tensor("wout", [128, 16 * D], F32, kind="ExternalInput").ap()
    ln_d = nc.dram_tensor("lnp", [128, 4 * D], F32, kind="ExternalInput").ap()
    wr_d = nc.dram_tensor("wr", [128, 16 * 36], F32, kind="ExternalInput").ap()
    br_d = nc.dram_tensor("br", [128, 36], F32, kind="ExternalInput").ap()
    wg_d = nc.dram_tensor("wg", [32, 128, 8192], F32, kind="ExternalInput").ap()
    wu_d = nc.dram_tensor("wu", [32, 128, 8192], F32, kind="ExternalInput").ap()
    wd_d = nc.dram_tensor("wd", [32, 128, 8192], F32, kind="ExternalInput").ap()
    ident_d = nc.dram_tensor("ident", [128, 128], F32, kind="ExternalInput").ap()
    out_d = nc.dram_tensor("out", [TT, D], F32, kind="ExternalOutput").ap()
    x1_scr = nc.dram_tensor("x1_scr", [TT, D], F32).ap()
    b_scr = [Buf() for _ in range(NT)]

    with ExitStack() as es:
        kb = KB(nc, es)
        pe, act, dve, pool, sp = kb.pe, kb.act, kb.dve, kb.pool, kb.sp
        psb = [kb.ps("ps%d" % i, [128, 512]) for i in range(7)]
        b_ps = [Buf() for _ in range(7)]
        pT = kb.ps("pT", [128, 8, 128], BF16)

        ident = kb.sb("ident", [128, 128], F32)
        ones128 = kb.sb("ones128", [128, 128], BF16)
        x1Tb = kb.sb("x1Tb", [128, 16, TT], BF16)
        identb = kb.sb("identb", [128, 128], BF16)
        b_identb, b_pT = Buf(), Buf()
        coef = kb.sb("coef", [128, NT * 32], F32)
        b_ident, b_ones, b_coef, b_lnp2 = Buf(), Buf(), Buf(), Buf()
        b_x1T = [Buf() for _ in range(NT)]
        kb.dma(sp, ident[:], ident_d[:, :], writes=[b_ident])
        kb.op(dve, lambda: nc.vector.memset(ones128[:], 1.0), writes=[b_ones])
        kb.op(dve, lambda: nc.vector.tensor_copy(out=identb[:], in_=ident[:]), reads=[b_ident], writes=[b_identb])

        with ExitStack() as esB:
            kb.es = esB
            mT = kb.sb("mT", [128, 16 * TT], BF16)
            b_mT = [Buf() for _ in range(16)]
            with ExitStack() as esA:
                kb.es = esA
                xTb = kb.sb("xTb", [128, 16 * TT], BF16)
                ofTb = kb.sb("ofTb", [128, 8 * TT], BF16)
                dnT = kb.sb("dnT", [128, 8 * TT], BF16)
                lamv = kb.sb("lamv", [128, 512], F32)
                gs = kb.sb("gs", [128, 8], F32)
                prod = kb.sb("prod", [128, 128], F32)
                sv = kb.sb("sv", [128, 4], F32)
                neglam = kb.sb("neglam", [128, 1], F32)
                o1s = kb.sb("o1s", [128, TT], F32)
                o2s = kb.sb("o2s", [128, TT], F32)
                dd = [kb.sb("dd%d" % i, [128, TT], F32) for i in range(2)]
                sq = [kb.sb("sq%d" % i, [128, TT], BF16) for i in range(2)]
                vv = kb.sb("vv", [128, 512], F32)
                slab = [kb.sb("slab%d" % i, [128, 6144], BF16) for i in range(2)]
                sgd = kb.sb("sgd", [128, 512], F32)
                sgf = kb.sb("sgf", [128, 512], F32)
                m1 = kb.sb("m1", [128, 512], F32)
                m2 = kb.sb("m2", [128, 512], F32)
                b_xTb, b_ofTb, b_dnT, b_lamv, b_gs, b_prod, b_sv, b_neglam = (Buf() for _ in range(8))
                b_o1s, b_o2s, b_vv, b_sgd, b_sgf, b_m1, b_m2 = (Buf() for _ in range(7))
                b_dd = [Buf(), Buf()]
                b_sq = [Buf(), Buf()]
                b_slab = [Buf(), Buf()]

                for k in range(16):
                    kb.dma(pool, xTb[:, k * TT:(k + 1) * TT], xT_d[:, k * TT:(k + 1) * TT], writes=[b_xTb])
                for k in range(8):
                    kb.dma(pool, ofTb[:, k * TT:(k + 1) * TT], ofT_d[:, k * TT:(k + 1) * TT], writes=[b_ofTb])
                kb.dma(sp, lamv[:], lamv_d[:, :], writes=[b_lamv])
                kb.dma(sp, gs[:], gcol_d[:, :], writes=[b_gs])

                def load_slab(j):
                    for q in range(3):
                        kb.dma(pool, slab[j % 2][:, q * 2048:(q + 1) * 2048], wmix_d[j][:, q * 2048:(q + 1) * 2048],
                               writes=[b_slab[j % 2]])
                load_slab(0)

                for j in range(2 if 'A1' not in SKIP else 0):
                    kb.op(dve, lambda j=j: nc.vector.tensor_tensor(out=prod[:], in0=lamv[:, (2 * j) * 128:(2 * j + 1) * 128],
                                                                  in1=lamv[:, (2 * j + 1) * 128:(2 * j + 2) * 128], op=ALU.mult),
                          reads=[b_lamv], writes=[b_prod])
                    kb.op(dve, lambda j=j: nc.vector.reduce_sum(out=sv[:, j:j + 1], in_=prod[:], axis=AX.X),
                          reads=[b_prod], writes=[b_sv])
                kb.op(act, lambda: nc.scalar.activation(out=sv[:, 2:4], in_=sv[:, 0:2], func=AF.Exp), reads=[b_sv], writes=[b_sv])
                kb.op(dve, lambda: nc.vector.tensor_tensor(out=neglam[:], in0=sv[:, 3:4], in1=sv[:, 2:3], op=ALU.subtract),
                      reads=[b_sv], writes=[b_neglam])
                kb.op(dve, lambda: nc.vector.tensor_scalar(out=neglam[:], in0=neglam[:], scalar1=-LAM_INIT, scalar2=None,
                                                           op0=ALU.add), reads=[b_neglam], writes=[b_neglam])
                kb.op(dve, lambda: nc.vector.tensor_scalar(out=gs[:], in0=gs[:], scalar1=1.0 - LAM_INIT, scalar2=None,
                                                           op0=ALU.mult), reads=[b_gs], writes=[b_gs])
                for h in range(4 if 'A2' not in SKIP else 0):
                    for i in range(2):
                        ch = h * 2 + i
                        kb.dma(sp, o1s[:], o1T_d[:, ch * TT:(ch + 1) * TT], writes=[b_o1s])
                        kb.dma(sp, o2s[:], o2T_d[:, ch * TT:(ch + 1) * TT], writes=[b_o2s])
                        kb.op(dve, lambda i=i: nc.vector.scalar_tensor_tensor(out=dd[i][:], in0=o2s[:], scalar=neglam[:, 0:1],
                                                                               in1=o1s[:], op0=ALU.mult, op1=ALU.add),
                              reads=[b_o1s, b_o2s, b_neglam], writes=[b_dd[i]])
                        kb.op(dve, lambda i=i: nc.vector.tensor_tensor(out=sq[i][:], in0=dd[i][:], in1=dd[i][:], op=ALU.mult),
                              reads=[b_dd[i]], writes=[b_sq[i]])
                    for hf in range(NH):
                        cs_ = slice(hf * 512, (hf + 1) * 512)
                        pb, bpb = psb[hf % 2], b_ps[hf % 2]
                        for i in range(2):
                            kb.op(pe, lambda i=i: nc.tensor.matmul(pb[:, :], lhsT=ones128[:], rhs=sq[i][:, cs_],
                                                                   start=(i == 0), stop=(i == 1)),
                                  reads=[b_ones, b_sq[i]], writes=[bpb])
                        kb.op(dve, lambda: nc.vector.tensor_scalar(out=vv[:], in0=pb[:, :], scalar1=1.0 / 256.0, scalar2=LN_EPS,
                                                                   op0=ALU.mult, op1=ALU.add), reads=[bpb], writes=[b_vv])
                        kb.op(act, lambda: nc.scalar.activation(out=vv[:], in_=vv[:], func=AF.Sqrt), reads=[b_vv], writes=[b_vv])
                        kb.op(dve, lambda: nc.vector.reciprocal(out=vv[:], in_=vv[:]), reads=[b_vv], writes=[b_vv])
                        for i in range(2):
                            ch = h * 2 + i
                            kb.op(dve, lambda i=i, ch=ch: nc.vector.scalar_tensor_tensor(
                                out=dnT[:, ch * TT + hf * 512:ch * TT + (hf + 1) * 512], in0=dd[i][:, cs_],
                                scalar=gs[:, ch:ch + 1], in1=vv[:], op0=ALU.mult, op1=ALU.mult),
                                reads=[b_dd[i], b_gs, b_vv], writes=[b_dnT])
                it = 0
                for j in range(16 if 'A3' not in SKIP else 0):
                    if j + 1 < 16:
                        load_slab(j + 1)
                    sl = slab[j % 2]
                    bsl = b_slab[j % 2]
                    for hf in range(NH):
                        st = 2 * (it % 2)
                        it += 1
                        pgd, pud, pgf, puf = psb[st], psb[st + 1], psb[4], psb[5]
                        bgd, bud, bgf, buf_ = b_ps[st], b_ps[st + 1], b_ps[4], b_ps[5]
                        for k in range(16):
                            kb.op(pe, lambda k=k: nc.tensor.matmul(pgd[:, :], lhsT=sl[:, k * 128:(k + 1) * 128],
                                                                   rhs=xTb[:, k * TT + hf * 512:k * TT + (hf + 1) * 512],
                                                                   start=(k == 0), stop=(k == 15)),
                                  reads=[bsl, b_xTb], writes=[bgd])
                        for k in range(16):
                            kb.op(pe, lambda k=k: nc.tensor.matmul(pgf[:, :], lhsT=sl[:, (16 + k) * 128:(17 + k) * 128],
                                                                   rhs=xTb[:, k * TT + hf * 512:k * TT + (hf + 1) * 512],
                                                                   start=(k == 0), stop=(k == 15)),
                                  reads=[bsl, b_xTb], writes=[bgf])
                        for k in range(8):
                            kb.op(pe, lambda k=k: nc.tensor.matmul(pud[:, :], lhsT=sl[:, (32 + k) * 128:(33 + k) * 128],
                                                                   rhs=dnT[:, k * TT + hf * 512:k * TT + (hf + 1) * 512],
                                                                   start=(k == 0), stop=(k == 7)),
                                  reads=[bsl, b_dnT], writes=[bud])
                        for k in range(8):
                            kb.op(pe, lambda k=k: nc.tensor.matmul(puf[:, :], lhsT=sl[:, (40 + k) * 128:(41 + k) * 128],
                                                                   rhs=ofTb[:, k * TT + hf * 512:k * TT + (hf + 1) * 512],
                                                                   start=(k == 0), stop=(k == 7)),
                                  reads=[bsl, b_ofTb], writes=[buf_])
                        kb.op(act, lambda: nc.scalar.activation(out=sgd[:], in_=pgd[:, :], func=AF.Sigmoid),
                              reads=[bgd], writes=[b_sgd])
                        kb.op(act, lambda: nc.scalar.activation(out=sgf[:], in_=pgf[:, :], func=AF.Sigmoid),
                              reads=[bgf], writes=[b_sgf])
                        kb.op(dve, lambda: nc.vector.tensor_tensor(out=m1[:], in0=sgd[:], in1=pud[:, :], op=ALU.mult),
                              reads=[b_sgd, bud], writes=[b_m1])
                        kb.op(dve, lambda: nc.vector.tensor_tensor(out=m2[:], in0=sgf[:], in1=puf[:, :], op=ALU.mult),
                              reads=[b_sgf, buf_], writes=[b_m2])
                        kb.op(dve, lambda: nc.vector.tensor_tensor(out=mT[:, j * TT + hf * 512:j * TT + (hf + 1) * 512],
                                                                   in0=m1[:], in1=m2[:], op=ALU.add),
                              reads=[b_m1, b_m2], writes=[b_mT[j]])
                kb.barrier()
                kb.es = esB
            woutb = kb.sb("woutb", [128, 16 * D], BF16)
            lnp1 = kb.sb("lnp1", [128, 2 * D], F32)
            wr = kb.sb("wr", [128, 16 * 36], F32)
            br = kb.sb("br", [128, 36], F32)
            xt = [kb.sb("xt%d" % i, [128, D], F32) for i in range(2)]
            yv = kb.sb("yv", [128, D], F32)
            x1t = kb.sb("x1t", [128, D], F32)
            xhi = kb.sb("xhi", [128, D], BF16)
            xlo = kb.sb("xlo", [128, D], BF16)
            x1Tl = kb.sb("x1Tl", [128, 16, 128], BF16)
            wrh = kb.sb("wrh", [128, 16 * 36], BF16)
            wrl = kb.sb("wrl", [128, 16 * 36], BF16)
            wrt = kb.sb("wrt", [128, 16 * 36], F32)
            b_xhi, b_xlo, b_x1Tl, b_wrh, b_wrl, b_wrt = (Buf() for _ in range(6))
            stats = kb.sb("stats", [128, 4 * 6], F32)
            mv = kb.sb("mv", [128, 2], F32)
            rt = kb.sb("rt", [128, 64], F32)
            lg = kb.sb("lg", [128, 36], F32)
            lem = kb.sb("lem", [128, 32], F32)
            lem2 = kb.sb("lem2", [128, 32], F32)
            oh1 = kb.sb("oh1", [128, 32], F32)
            oh2 = kb.sb("oh2", [128, 32], F32)
            b_wout, b_lnp1, b_wr, b_br, b_yv, b_x1t, b_x1Tf_unused, b_stats, b_mv, b_rt, b_lg, b_lem, b_lem2, b_oh1, b_oh2 = (
                Buf() for _ in range(15))
            b_xt = [Buf(), Buf()]
            for k in range(16):
                kb.dma(pool, woutb[:, k * D:(k + 1) * D], wout_d[:, k * D:(k + 1) * D], writes=[b_wout])
            kb.dma(sp, lnp1[:], ln_d[:, 0:2 * D], writes=[b_lnp1])
            kb.dma(sp, wr[:], wr_d[:, :], writes=[b_wr])
            kb.dma(sp, br[:], br_d[:, :], writes=[b_br])
            kb.op(dve, lambda: nc.vector.tensor_copy(out=wrh[:], in_=wr[:]), reads=[b_wr], writes=[b_wrh])
            kb.op(dve, lambda: nc.vector.tensor_tensor(out=wrt[:], in0=wr[:], in1=wrh[:], op=ALU.subtract),
                  reads=[b_wr, b_wrh], writes=[b_wrt])
            kb.op(dve, lambda: nc.vector.tensor_copy(out=wrl[:], in_=wrt[:]), reads=[b_wrt], writes=[b_wrl])
            kb.dma(sp, xt[0][:], x_d[0:128, :], writes=[b_xt[0]])
            for tb in range(NT if 'B' not in SKIP else 0):
                if tb + 1 < NT:
                    kb.dma(sp, xt[(tb + 1) % 2][:], x_d[(tb + 1) * 128:(tb + 2) * 128, :], writes=[b_xt[(tb + 1) % 2]])
                X = xt[tb % 2]
                bX = b_xt[tb % 2]
                for n in range(4):
                    pb, bpb = psb[n % 2], b_ps[n % 2]
                    for k in range(16):
                        kb.op(pe, lambda k=k: nc.tensor.matmul(pb[:, :], lhsT=mT[:, k * TT + tb * 128:k * TT + (tb + 1) * 128],
                                                               rhs=woutb[:, k * D + n * 512:k * D + (n + 1) * 512],
                                                               start=(k == 0), stop=(k == 15)),
                              reads=[b_mT[k], b_wout], writes=[bpb])
                    kb.op(dve, lambda n=n: nc.vector.scalar_tensor_tensor(out=yv[:, n * 512:(n + 1) * 512],
                                                                         in0=X[:, n * 512:(n + 1) * 512], scalar=ALPHA,
                                                                         in1=pb[:, :], op0=ALU.mult, op1=ALU.add),
                          reads=[bX, bpb], writes=[b_yv])
                    kb.op(dve, lambda n=n: nc.vector.bn_stats(out=stats[:, n * 6:(n + 1) * 6], in_=yv[:, n * 512:(n + 1) * 512]),
                          reads=[b_yv], writes=[b_stats])
                _ln_tail(nc, kb, yv, b_yv, stats, b_stats, mv, b_mv, x1t, b_x1t, lnp1, b_lnp1)
                kb.dma(sp, x1_scr[tb * 128:(tb + 1) * 128, :], x1t[:], reads=[b_x1t], writes=[b_scr[tb]])
                kb.op(dve, lambda: nc.vector.tensor_copy(out=xhi[:], in_=x1t[:]), reads=[b_x1t], writes=[b_xhi])
                kb.op(dve, lambda: nc.vector.tensor_tensor(out=xlo[:], in0=x1t[:], in1=xhi[:], op=ALU.subtract),
                      reads=[b_x1t, b_xhi], writes=[b_xlo])
                for si, (src, bsrc) in enumerate(((xhi, b_xhi), (xlo, b_xlo))):
                    for k8 in range(2):
                        for kk in range(8):
                            k = k8 * 8 + kk
                            kb.op(pe, lambda k=k, kk=kk: nc.tensor.transpose(pT[:, kk, :], src[:, k * 128:(k + 1) * 128], identb[:]),
                                  reads=[bsrc, b_identb], writes=[b_pT])
                        if si == 0:
                            kb.op(act, lambda: nc.scalar.activation(out=x1Tb[:, k8 * 8:(k8 + 1) * 8, tb * 128:(tb + 1) * 128],
                                                                    in_=pT[:, :, :], func=AF.Copy),
                                  reads=[b_pT], writes=[b_x1T[tb]])
                        else:
                            kb.op(dve, lambda: nc.vector.tensor_copy(out=x1Tl[:, k8 * 8:(k8 + 1) * 8, :], in_=pT[:, :, :]),
                                  reads=[b_pT], writes=[b_x1Tl])
                pr, bpr = psb[6], b_ps[6]
                for k in range(16):
                    hi_k = x1Tb[:, k, tb * 128:(tb + 1) * 128]
                    kb.op(pe, lambda: nc.tensor.matmul(pr[:, 0:36], lhsT=hi_k, rhs=wrh[:, k * 36:(k + 1) * 36],
                                                       start=(k == 0), stop=False), reads=[b_x1T[tb], b_wrh], writes=[bpr])
                    kb.op(pe, lambda: nc.tensor.matmul(pr[:, 0:36], lhsT=hi_k, rhs=wrl[:, k * 36:(k + 1) * 36],
                                                       start=False, stop=False), reads=[b_x1T[tb], b_wrl], writes=[bpr])
                    kb.op(pe, lambda: nc.tensor.matmul(pr[:, 0:36], lhsT=x1Tl[:, k, :], rhs=wrh[:, k * 36:(k + 1) * 36],
                                                       start=False, stop=(k == 15)), reads=[b_x1Tl, b_wrh], writes=[bpr])
                if 'B4' in SKIP:
                    continue
                V = nc.vector
                R = lambda a, b=None: rt[:, a:(b if b is not None else a + 1)]
                kb.op(dve, lambda: V.tensor_tensor(out=lg[:], in0=pr[:, 0:36], in1=br[:], op=ALU.add), reads=[bpr, b_br], writes=[b_lg])
                seq = [
                    lambda: V.reduce_max(out=R(0), in_=lg[:, 0:4], axis=AX.X),
                    lambda: V.tensor_scalar(out=R(4, 8), in0=lg[:, 0:4], scalar1=R(0), scalar2=None, op0=ALU.is_equal),
                    lambda: V.tensor_scalar(out=R(8, 12), in0=lg[:, 0:4], scalar1=R(0), scalar2=None, op0=ALU.subtract),
                ]
                for f in seq:
                    kb.op(dve, f, reads=[b_lg, b_rt], writes=[b_rt])
                kb.op(act, lambda: nc.scalar.activation(out=R(8, 12), in_=R(8, 12), func=AF.Exp), reads=[b_rt], writes=[b_rt])
                seq = [
                    lambda: V.reduce_sum(out=R(1), in_=R(8, 12), axis=AX.X),
                    lambda: V.reciprocal(out=R(2), in_=R(1)),
                    lambda: V.tensor_scalar(out=R(12, 16), in0=R(4, 8), scalar1=-1.0, scalar2=1e30, op0=ALU.add, op1=ALU.mult),
                ]
                for f in seq:
                    kb.op(dve, f, reads=[b_rt], writes=[b_rt])
                for g in range(4):
                    kb.op(dve, lambda g=g: V.tensor_scalar(out=lem[:, g * 8:(g + 1) * 8], in0=lg[:, 4 + g * 8:12 + g * 8],
                                                           scalar1=R(12 + g), scalar2=None, op0=ALU.add),
                          reads=[b_lg, b_rt], writes=[b_lem])
                kb.op(dve, lambda: V.reduce_max(out=R(16), in_=lem[:], axis=AX.X), reads=[b_lem, b_rt], writes=[b_rt])
                kb.op(dve, lambda: V.tensor_scalar(out=oh1[:], in0=lem[:], scalar1=R(16), scalar2=None, op0=ALU.is_equal),
                      reads=[b_lem, b_rt], writes=[b_oh1])
                kb.op(dve, lambda: V.scalar_tensor_tensor(out=lem2[:], in0=oh1[:], scalar=-1e30, in1=lem[:], op0=ALU.mult, op1=ALU.add),
                      reads=[b_oh1, b_lem], writes=[b_lem2])
                kb.op(dve, lambda: V.reduce_max(out=R(17), in_=lem2[:], axis=AX.X), reads=[b_lem2, b_rt], writes=[b_rt])
                kb.op(dve, lambda: V.tensor_scalar(out=oh2[:], in0=lem2[:], scalar1=R(17), scalar2=None, op0=ALU.is_equal),
                      reads=[b_lem2, b_rt], writes=[b_oh2])
                kb.op(dve, lambda: V.tensor_tensor(out=R(18), in0=R(17), in1=R(16), op=ALU.subtract), reads=[b_rt], writes=[b_rt])
                kb.op(act, lambda: nc.scalar.activation(out=R(19), in_=R(18), func=AF.Exp), reads=[b_rt], writes=[b_rt])
                seq = [
                    lambda: V.tensor_scalar(out=R(20), in0=R(19), scalar1=1.0, scalar2=None, op0=ALU.add),
                    lambda: V.reciprocal(out=R(21), in_=R(20)),
                    lambda: V.tensor_tensor(out=R(22), in0=R(19), in1=R(21), op=ALU.mult),
                    lambda: V.tensor_tensor(out=R(23), in0=R(21), in1=R(2), op=ALU.mult),
                    lambda: V.tensor_tensor(out=R(24), in0=R(22), in1=R(2), op=ALU.mult),
                ]
                for f in seq:
                    kb.op(dve, f, reads=[b_rt], writes=[b_rt])
                cf = coef[:, tb * 32:(tb + 1) * 32]
                kb.op(dve, lambda: V.tensor_scalar(out=cf, in0=oh1[:], scalar1=R(23), scalar2=None, op0=ALU.mult),
                      reads=[b_oh1, b_rt], writes=[b_coef])
                kb.op(dve, lambda: V.scalar_tensor_tensor(out=cf, in0=oh2[:], scalar=R(24), in1=cf, op0=ALU.mult, op1=ALU.add),
                      reads=[b_oh2, b_rt, b_coef], writes=[b_coef])
            kb.barrier()
            kb.es = es
        yacc = kb.sb("yacc", [128, NT * D], F32)
        esM = ExitStack()
        kb.es = esM
        wgb = [kb.sb("wgb%d" % i, [128, 8192], BF16) for i in range(2)]
        wub = [kb.sb("wub%d" % i, [128, 8192], BF16) for i in range(2)]
        wdb = kb.sb("wdb", [128, 8192], BF16)
        hT = kb.sb("hT", [128, 4 * TT], BF16)
        sgl = [kb.sb("sgl%d" % i, [128, 512], F32) for i in range(2)]
        b_wg = [Buf(), Buf()]
        b_wu = [Buf(), Buf()]
        b_wd, b_hT, b_yacc = Buf(), Buf(), Buf()
        b_sgl = [Buf(), Buf()]
        b_ya = [Buf() for _ in range(NT)]

        def load_gu(e):
            for q in range(4):
                kb.dma(pool, wgb[e % 2][:, q * 2048:(q + 1) * 2048], wg_d[e][:, q * 2048:(q + 1) * 2048], writes=[b_wg[e % 2]])
            for q in range(4):
                kb.dma(pool, wub[e % 2][:, q * 2048:(q + 1) * 2048], wu_d[e][:, q * 2048:(q + 1) * 2048], writes=[b_wu[e % 2]])

        def load_d(e):
            for q in range(4):
                kb.dma(pool, wdb[:, q * 2048:(q + 1) * 2048], wd_d[e][:, q * 2048:(q + 1) * 2048], writes=[b_wd])

        load_gu(0)
        it = 0
        for e in range(32 if 'M' not in SKIP else 0):
            load_d(e)
            if e + 1 < 32:
                load_gu(e + 1)
            G, U = wgb[e % 2], wub[e % 2]
            for hf in range(NH):
                for j in range(4):
                    st = 2 * (it % 2)
                    it += 1
                    pg, pu = psb[st], psb[st + 1]
                    bpg, bpu = b_ps[st], b_ps[st + 1]
                    for k in range(16):
                        kb.op(pe, lambda k=k: nc.tensor.matmul(pg[:, :], lhsT=G[:, k * 512 + j * 128:k * 512 + (j + 1) * 128],
                                                               rhs=x1Tb[:, k, hf * 512:(hf + 1) * 512],
                                                               start=(k == 0), stop=(k == 15)),
                              reads=[b_wg[e % 2]] + b_x1T, writes=[bpg])
                    for k in range(16):
                        kb.op(pe, lambda k=k: nc.tensor.matmul(pu[:, :], lhsT=U[:, k * 512 + j * 128:k * 512 + (j + 1) * 128],
                                                               rhs=x1Tb[:, k, hf * 512:(hf + 1) * 512],
                                                               start=(k == 0), stop=(k == 15)),
                              reads=[b_wu[e % 2]] + b_x1T, writes=[bpu])
                    sg = sgl[it % 2]
                    bsg = b_sgl[it % 2]
                    kb.op(act, lambda: nc.scalar.activation(out=sg[:], in_=pg[:, :], func=AF.Silu), reads=[bpg], writes=[bsg])
                    kb.op(dve, lambda: nc.vector.tensor_tensor(out=hT[:, j * TT + hf * 512:j * TT + (hf + 1) * 512],
                                                               in0=sg[:], in1=pu[:, :], op=ALU.mult),
                          reads=[bsg, bpu], writes=[b_hT])
            for tb in range(NT):
                for n in range(4):
                    pd, bpd = psb[4 + (tb * 4 + n) % 3], b_ps[4 + (tb * 4 + n) % 3]
                    for j in range(4):
                        kb.op(pe, lambda j=j: nc.tensor.matmul(pd[:, :], lhsT=hT[:, j * TT + tb * 128:j * TT + (tb + 1) * 128],
                                                               rhs=wdb[:, j * 2048 + n * 512:j * 2048 + (n + 1) * 512],
                                                               start=(j == 0), stop=(j == 3)),
                              reads=[b_hT, b_wd], writes=[bpd])
                    ya = yacc[:, tb * D + n * 512:tb * D + (n + 1) * 512]
                    csc = coef[:, tb * 32 + e:tb * 32 + e + 1]
                    if e == 0:
                        kb.op(dve, lambda: nc.vector.tensor_scalar(out=ya, in0=pd[:, :], scalar1=csc, scalar2=None, op0=ALU.mult),
                              reads=[bpd, b_coef], writes=[b_ya[tb]])
                    else:
                        kb.op(dve, lambda: nc.vector.scalar_tensor_tensor(out=ya, in0=pd[:, :], scalar=csc, in1=ya,
                                                                          op0=ALU.mult, op1=ALU.add),
                              reads=[bpd, b_coef, b_ya[tb]], writes=[b_ya[tb]])
        kb.barrier()
        esM.close()
        kb.es = es
        lnp2 = kb.sb("lnp2", [128, 2 * D], F32)
        kb.dma(sp, lnp2[:], ln_d[:, 2 * D:4 * D], writes=[b_lnp2])
        x1r = [kb.sb("x1r%d" % i, [128, D], F32) for i in range(2)]
        b_x1r = [Buf(), Buf()]
        y2 = kb.sb("y2", [128, D], F32)
        o2 = [kb.sb("o2_%d" % i, [128, D], F32) for i in range(2)]
        stats2 = kb.sb("stats2", [128, 24], F32)
        mv2 = kb.sb("mv2", [128, 2], F32)
        b_y2, b_stats2, b_mv2 = Buf(), Buf(), Buf()
        b_o2 = [Buf(), Buf()]
        for tb in range(NT):
            r = tb % 2
            kb.dma(sp, x1r[r][:], x1_scr[tb * 128:(tb + 1) * 128, :], reads=[b_scr[tb]], writes=[b_x1r[r]])
            for n in range(4):
                kb.op(dve, lambda n=n: nc.vector.scalar_tensor_tensor(out=y2[:, n * 512:(n + 1) * 512],
                                                                     in0=x1r[r][:, n * 512:(n + 1) * 512], scalar=ALPHA,
                                                                     in1=yacc[:, tb * D + n * 512:tb * D + (n + 1) * 512],
                                                                     op0=ALU.mult, op1=ALU.add),
                      reads=[b_x1r[r], b_ya[tb]], writes=[b_y2])
                kb.op(dve, lambda n=n: nc.vector.bn_stats(out=stats2[:, n * 6:(n + 1) * 6], in_=y2[:, n * 512:(n + 1) * 512]),
                      reads=[b_y2], writes=[b_stats2])
            _ln_tail(nc, kb, y2, b_y2, stats2, b_stats2, mv2, b_mv2, o2[r], b_o2[r], lnp2, b_lnp2)
            tok = kb.dma(sp, out_d[tb * 128:(tb + 1) * 128, :], o2[r][:], reads=[b_o2[r]])
            kb.out_toks.append(tok)
        for tok in kb.out_toks:
            sp.need(tok)
    return nc


def _ln_tail(nc, kb, yv, b_yv, stats, b_stats, mv, b_mv, outt, b_out, lnp, b_lnp):
    dve, act = kb.dve, kb.act
    V = nc.vector
    kb.op(dve, lambda: V.bn_aggr(out=mv[:], in_=stats[:]), reads=[b_stats], writes=[b_mv])
    kb.op(dve, lambda: V.tensor_scalar(out=mv[:, 1:2], in0=mv[:, 1:2], scalar1=LN_EPS, scalar2=None, op0=ALU.add),
          reads=[b_mv], writes=[b_mv])
    kb.op(act, lambda: nc.scalar.activation(out=mv[:, 1:2], in_=mv[:, 1:2], func=AF.Sqrt), reads=[b_mv], writes=[b_mv])
    kb.op(dve, lambda: V.reciprocal(out=mv[:, 1:2], in_=mv[:, 1:2]), reads=[b_mv], writes=[b_mv])
    kb.op(dve, lambda: V.tensor_scalar(out=outt[:], in0=yv[:], scalar1=mv[:, 0:1], scalar2=mv[:, 1:2],
                                       op0=ALU.subtract, op1=ALU.mult), reads=[b_yv, b_mv], writes=[b_out])
    kb.op(dve, lambda: V.tensor_tensor(out=outt[:], in0=outt[:], in1=lnp[:, 0:D], op=ALU.mult), reads=[b_out, b_lnp], writes=[b_out])
    kb.op(dve, lambda: V.tensor_tensor(out=outt[:], in0=outt[:], in1=lnp[:, D:2 * D], op=ALU.add), reads=[b_out, b_lnp], writes=[b_out])


def _rep(v, n=128):
    v = np.asarray(v, np.float32).reshape(1, -1)
    return np.ascontiguousarray(np.repeat(v, n, 0))


def l2_weights(inp):
    w = np.asarray(inp["w_in"], np.float32).reshape(D, -1)
    wgate = w[:, 6152:6152 + 4096]
    wpd = np.asarray(inp["w_proj_diff"], np.float32).reshape(1024, D)
    wpf = np.asarray(inp["w_proj_fox"], np.float32).reshape(1024, D)
    slabs = []
    for j in range(16):
        cols = slice(j * 128, (j + 1) * 128)
        gd = wgate[:, j * 128:(j + 1) * 128].reshape(16, 128, 128)
        gf = wgate[:, 2048 + j * 128:2048 + (j + 1) * 128].reshape(16, 128, 128)
        pd = wpd[:, cols].reshape(8, 128, 128)
        pf = wpf[:, cols].reshape(8, 128, 128)
        sl = np.concatenate([gd, gf, pd, pf], 0)
        slabs.append(sl.transpose(1, 0, 2).reshape(128, 48 * 128))
    wmix = np.ascontiguousarray(np.stack(slabs, 0))
    wout = _pk(np.asarray(inp["w_out"], np.float32).reshape(D, D))
    lnp = np.concatenate([_rep(inp["ln1_g"]), _rep(inp["ln1_b"]), _rep(inp["ln2_g"]), _rep(inp["ln2_b"])], 1)
    wrg = np.asarray(inp["w_router_group"], np.float32).reshape(D, 4)
    wre = np.asarray(inp["w_router_expert"], np.float32).reshape(4, D, 8).transpose(1, 0, 2).reshape(D, 32)
    wr = _pk(np.concatenate([wrg, wre], 1))
    br = _rep(np.concatenate([np.asarray(inp["b_router_group"], np.float32).reshape(-1),
                              np.asarray(inp["b_router_expert"], np.float32).reshape(-1)]))
    wg = np.asarray(inp["w_gate"], np.float32).reshape(32, 16, 128, 512).transpose(0, 2, 1, 3).reshape(32, 128, 8192)
    wu = np.asarray(inp["w_up"], np.float32).reshape(32, 16, 128, 512).transpose(0, 2, 1, 3).reshape(32, 128, 8192)
    wd = np.asarray(inp["w_down"], np.float32).reshape(32, 4, 128, D).transpose(0, 2, 1, 3).reshape(32, 128, 8192)
    lamv = np.concatenate([_rep(inp["lam_q1"]), _rep(inp["lam_k1"]), _rep(inp["lam_q2"]), _rep(inp["lam_k2"])], 1)
    gcol = np.ascontiguousarray(np.asarray(inp["diff_norm_g"], np.float32).reshape(8, 128).T)
    return {"lamv": lamv, "gcol": gcol, "wmix": wmix, "wout": wout, "lnp": np.ascontiguousarray(lnp), "wr": wr, "br": br,
            "wg": np.ascontiguousarray(wg), "wu": np.ascontiguousarray(wu), "wd": np.ascontiguousarray(wd),
            "ident": np.eye(128, dtype=np.float32)}


def _fm(a, TT):
    nf = a.shape[1] // 128
    return np.ascontiguousarray(a.reshape(TT, nf, 128).transpose(2, 1, 0).reshape(128, nf * TT))


def run_l2(inp, x2, o_f, o_d, TT, ncore):
    wts = l2_weights(inp)
    in_maps = []
    for c in range(ncore):
        tsl = slice(c * TT, (c + 1) * TT)
        xc = np.ascontiguousarray(x2[tsl])
        o1 = np.concatenate([o_d[2 * h][tsl] for h in range(4)], 1)
        o2 = np.concatenate([o_d[2 * h + 1][tsl] for h in range(4)], 1)
        of = np.concatenate([o_f[h][tsl] for h in range(8)], 1)
        m = dict(wts)
        m.update({"xT": _fm(xc, TT), "x": xc, "o1T": _fm(o1, TT), "o2T": _fm(o2, TT), "ofT": _fm(of, TT)})
        in_maps.append(m)
    nc = build_l2(TT)
    res = run_bass_kernel_spmd(nc, in_maps, core_ids=list(range(ncore)))
    return np.concatenate([r["out"] for r in res.results], 0)


def kernel(**inp):
    x = np.asarray(inp["x"], np.float32)
    o_f, o_d = run_l1(inp["x"], inp["w_in"], inp["b_forget"])
    out = run_l2(inp, x.reshape(S, D), o_f, o_d, S // NCORE, NCORE)
    return out.reshape(1, S, D).astype(np.float32)
```
